# Optimizing a Trainium2 kernel written in Bass

```python
import math
import jax, jax.numpy as jnp
from jax import lax
import numpy as np

D_MODEL = 1024
BATCH = 8
SEQ = 8192
DEPTH = 1

EPS = 1e-6
NEG_INF = -1e30

POOL_WINDOWS = (2, 4, 8, 16)
POOL_GROUPS = len(POOL_WINDOWS)
POOL_IN = 128
POOL_WIDTH = POOL_GROUPS * POOL_IN
POOL_OUT = D_MODEL // POOL_GROUPS

ATT_HEADS = 16
HEAD_DIM = D_MODEL // ATT_HEADS
DILATED_PATTERNS = ((128, 1), (512, 4), (2048, 16))
N_PATTERNS = len(DILATED_PATTERNS)
Q_WIDTH = N_PATTERNS * ATT_HEADS * HEAD_DIM
KV_WIDTH = ATT_HEADS * HEAD_DIM

N_BRANCHES = 2
GATE_WIDTH = N_BRANCHES * D_MODEL
IN_WIDTH = POOL_WIDTH + Q_WIDTH + 2 * KV_WIDTH + GATE_WIDTH

PEER_HEADS = 8
PEER_NKEYS = 128
PEER_N = PEER_NKEYS * PEER_NKEYS
PEER_QDIM = 256
PEER_HALF = PEER_QDIM // 2
PEER_TOPK = 16
PEER_CHUNK = 128

kernel_name = "hybrid_pool_dilattn_peer_block"


def rms_norm(x, g):
    xf = x.astype(jnp.float32)
    y = xf * lax.rsqrt(jnp.mean(xf * xf, axis=-1, keepdims=True) + EPS)
    return (y * g.astype(jnp.float32)).astype(x.dtype)


def alibi_slopes(n):
    def geometric(k):
        start = 2.0 ** (-8.0 / k)
        return [start ** (i + 1) for i in range(k)]
    p = 2 ** int(math.floor(math.log2(n)))
    s = geometric(p) + geometric(2 * p)[0::2][: n - p]
    return np.sort(np.array(s, dtype=np.float32))[::-1].copy()


def pool_mixer(a, w_pool, pool_scale):
    B, S = a.shape[0], a.shape[1]
    af = a.astype(jnp.float32)
    cs = jnp.pad(jnp.cumsum(af, axis=1), ((0, 0), (1, 0), (0, 0), (0, 0)))
    t = jnp.arange(S)[:, None]
    win = jnp.array(POOL_WINDOWS, dtype=jnp.int32)[None, :]
    lo = jnp.maximum(t + 1 - win, 0)
    grp = jnp.arange(POOL_GROUPS)[None, :]
    window_sum = cs[:, 1:] - cs[:, lo, grp, :]
    count = jnp.minimum(t + 1, win).astype(jnp.float32)[None, :, :, None]
    d = (window_sum / count - af).astype(a.dtype)
    y = jnp.einsum('bsgc,gco->bsgo', d, w_pool)
    return y.reshape(B, S, D_MODEL) * pool_scale


def dilated_group(q, k, v, window, dilation, slopes):
    B, S, H, Dh = q.shape
    band = window // dilation
    n = -(-S // (dilation * band)) * band
    L = n * dilation
    nb = n // band

    def blocks(x):
        x = jnp.pad(x, ((0, 0), (0, L - S), (0, 0), (0, 0)))
        return x.reshape(B, nb, band, dilation, H, Dh)

    def with_prev(xb):
        prev = jnp.pad(xb, ((0, 0), (1, 0), (0, 0), (0, 0), (0, 0), (0, 0)))[:, :nb]
        return jnp.concatenate([prev, xb], axis=2)

    qb = blocks(q)
    kb = with_prev(blocks(k))
    vb = with_prev(blocks(v))
    s = jnp.einsum('bnqrhe,bnkrhe->bnrhqk', qb, kb).astype(jnp.float32)
    qi = jnp.arange(band)[:, None]
    ki = jnp.arange(2 * band)[None, :]
    step = qi + band - ki
    key_idx = jnp.arange(nb)[:, None, None] * band + ki[None] - band
    valid = (step >= 0)[None] & (step <= band)[None] & (key_idx >= 0)
    alibi = -slopes[:, None, None] * (step * dilation).astype(jnp.float32)[None]
    s = jnp.where(valid[None, :, None, None], s + alibi[None, None, None], NEG_INF)
    m = jnp.max(s, axis=-1, keepdims=True)
    p = jnp.exp(s - m)
    den = jnp.sum(p, axis=-1)
    o = jnp.einsum('bnrhqk,bnkrhe->bnrhqe', p, vb.astype(jnp.float32)) / den[..., None]
    o = o.transpose(0, 1, 4, 2, 3, 5).reshape(B, L, H, Dh)[:, :S]
    m = m[..., 0].transpose(0, 1, 4, 2, 3).reshape(B, L, H)[:, :S]
    den = den.transpose(0, 1, 4, 2, 3).reshape(B, L, H)[:, :S]
    return o, m, den


def dilated_attention(q, k, v, slopes):
    outs, maxs, dens = [], [], []
    for g, (window, dilation) in enumerate(DILATED_PATTERNS):
        o, m, den = dilated_group(q[:, :, g], k, v, window, dilation, slopes[g])
        outs.append(o)
        maxs.append(m)
        dens.append(den)
    o = jnp.stack(outs)
    m = jnp.stack(maxs)
    den = jnp.stack(dens)
    w = den * jnp.exp(m - jnp.max(m, axis=0, keepdims=True))
    w = w / jnp.sum(w, axis=0, keepdims=True)
    return jnp.sum(w[..., None] * o, axis=0)


def peer(hn, w_query, sub_keys, expert_u, expert_v):
    B, S, D = hn.shape
    tokens = hn.reshape(-1, PEER_CHUNK, D)

    def chunk_fn(xc):
        C = xc.shape[0]
        q = (xc @ w_query).reshape(C, PEER_HEADS, 2, PEER_HALF)
        sc = jnp.einsum('chpe,hpne->chpn', q, sub_keys).astype(jnp.float32)
        top_s, top_i = lax.top_k(sc, PEER_TOPK)
        cand_s = top_s[:, :, 0, :, None] + top_s[:, :, 1, None, :]
        cand_i = top_i[:, :, 0, :, None] * PEER_NKEYS + top_i[:, :, 1, None, :]
        best_s, best_j = lax.top_k(cand_s.reshape(C, PEER_HEADS, -1), PEER_TOPK)
        experts = jnp.take_along_axis(cand_i.reshape(C, PEER_HEADS, -1), best_j, axis=-1)
        gate = jax.nn.softmax(best_s, axis=-1)
        u = expert_u[experts]
        v = expert_v[experts]
        act = jax.nn.gelu(jnp.einsum('cd,chkd->chk', xc, u).astype(jnp.float32))
        return jnp.einsum('chk,chkd->cd', (gate * act).astype(xc.dtype), v)

    return lax.map(chunk_fn, tokens).reshape(B, S, D)


def setup_inputs(seed: int = 0) -> dict:
    key = jax.random.key(seed)
    ks = jax.random.split(key, 13)
    nrm = jax.random.normal
    return {
        "x": nrm(ks[0], (BATCH, SEQ, D_MODEL), jnp.float32),
        "norm1_g": 1.0 + 0.05 * nrm(ks[1], (DEPTH, D_MODEL), jnp.float32),
        "w_in": nrm(ks[2], (DEPTH, D_MODEL, IN_WIDTH), jnp.float32) * D_MODEL ** -0.5,
        "q_norm_g": 1.0 + 0.05 * nrm(ks[3], (DEPTH, HEAD_DIM), jnp.float32),
        "k_norm_g": 1.0 + 0.05 * nrm(ks[4], (DEPTH, HEAD_DIM), jnp.float32),
        "w_pool": nrm(ks[5], (DEPTH, POOL_GROUPS, POOL_IN, POOL_OUT), jnp.float32) * POOL_IN ** -0.5,
        "pool_scale": 1.0 + 0.1 * nrm(ks[6], (DEPTH, D_MODEL), jnp.float32),
        "w_out": nrm(ks[7], (DEPTH, D_MODEL, D_MODEL), jnp.float32) * D_MODEL ** -0.5,
        "norm2_g": 1.0 + 0.05 * nrm(ks[8], (DEPTH, D_MODEL), jnp.float32),
        "w_query": nrm(ks[9], (DEPTH, D_MODEL, PEER_HEADS * PEER_QDIM), jnp.float32) * D_MODEL ** -0.5,
        "sub_keys": nrm(ks[10], (DEPTH, PEER_HEADS, 2, PEER_NKEYS, PEER_HALF), jnp.float32) * PEER_HALF ** -0.5,
        "expert_u": nrm(ks[11], (DEPTH, PEER_N, D_MODEL), jnp.float32) * D_MODEL ** -0.5,
        "expert_v": nrm(ks[12], (DEPTH, PEER_N, D_MODEL), jnp.float32) * PEER_HEADS ** -0.5,
    }


def reference(x, norm1_g, w_in, q_norm_g, k_norm_g, w_pool, pool_scale, w_out, norm2_g, w_query, sub_keys, expert_u, expert_v):
    B, S, D = x.shape
    slopes = jnp.asarray(alibi_slopes(N_PATTERNS * ATT_HEADS)).reshape(N_PATTERNS, ATT_HEADS)
    splits = [POOL_WIDTH, POOL_WIDTH + Q_WIDTH, POOL_WIDTH + Q_WIDTH + KV_WIDTH,
              POOL_WIDTH + Q_WIDTH + 2 * KV_WIDTH]
    h = x
    for l in range(DEPTH):
        xn = rms_norm(h, norm1_g[l])
        proj = xn @ w_in[l]
        a_in, q, k, v, gate_pre = jnp.split(proj, splits, axis=-1)
        pool_out = pool_mixer(a_in.reshape(B, S, POOL_GROUPS, POOL_IN), w_pool[l], pool_scale[l])
        q = rms_norm(q.reshape(B, S, N_PATTERNS, ATT_HEADS, HEAD_DIM), q_norm_g[l]) * (HEAD_DIM ** -0.5)
        k = rms_norm(k.reshape(B, S, ATT_HEADS, HEAD_DIM), k_norm_g[l])
        v = v.reshape(B, S, ATT_HEADS, HEAD_DIM)
        attn_out = dilated_attention(q, k, v, slopes).astype(h.dtype).reshape(B, S, D)
        gates = jax.nn.sigmoid(gate_pre.astype(jnp.float32)).astype(h.dtype).reshape(B, S, N_BRANCHES, D)
        merged = gates[:, :, 0] * pool_out + gates[:, :, 1] * attn_out
        h = h + merged @ w_out[l]
        hn = rms_norm(h, norm2_g[l])
        h = h + peer(hn, w_query[l], sub_keys[l], expert_u[l], expert_v[l])
    return h
```

```python
import numpy as np
from contextlib import ExitStack
import concourse.bass as bass
import concourse.mybir as mybir
from concourse.bass_utils import run_bass_kernel_spmd

F32 = mybir.dt.float32
BF16 = mybir.dt.bfloat16
U32 = mybir.dt.uint32
I32 = mybir.dt.int32
AF = mybir.ActivationFunctionType
ALU = mybir.AluOpType
AX = mybir.AxisListType

ENGS = ("pe", "dve", "act", "pool", "sp")
EPOCH = 20000


class Buf:
    __slots__ = ("w", "r", "name")

    def __init__(self, name=""):
        self.w = {}
        self.r = {}
        self.name = name


class Grp:
    __slots__ = ("key", "cnt")

    def __init__(self, key):
        self.key = key
        self.cnt = 0


class Prog:
    def __init__(self, nc, stack):
        self.nc = nc
        self.stack = stack
        self.semh = []
        self.ops = {e: [] for e in ENGS}
        self.cnt = {e: 0 for e in ENGS}
        self.key = {}
        self.ownkeys = {e: set() for e in ENGS}
        self.waited = {e: {} for e in ENGS}
        for e in ENGS:
            self._new_epoch(e)
        self.nops = 0
        self.latest = {}

    def barrier(self):
        for e in ENGS:
            waits = []
            for k, v in self.latest.items():
                if k in self.ownkeys[e]:
                    continue
                if self.waited[e].get(k, 0) >= v:
                    continue
                self.waited[e][k] = v
                waits.append((k, v))
            self.ops[e].append((waits, None, None))

    def _new_sem(self, name):
        h = self.stack.enter_context(self.nc.semaphore(name))
        self.semh.append(h)
        return len(self.semh) - 1

    def _new_epoch(self, e):
        k = self._new_sem("s_%s_%d" % (e, len(self.ownkeys[e])))
        self.key[e] = k
        self.ownkeys[e].add(k)
        self.cnt[e] = 0

    def grp(self, name="g"):
        return Grp(self._new_sem("d_%s_%d" % (name, len(self.semh))))

    def sb(self, name, shape, dt):
        return self.stack.enter_context(self.nc.sbuf_tensor(name, shape, dt))

    def ps(self, name, shape, dt):
        return self.stack.enter_context(self.nc.psum_tensor(name, shape, dt))

    def _deps(self, eng, reads, writes):
        deps = {}
        for b in reads:
            for k, v in b.w.items():
                if deps.get(k, 0) < v:
                    deps[k] = v
        for b in writes:
            for k, v in b.w.items():
                if deps.get(k, 0) < v:
                    deps[k] = v
            for k, v in b.r.items():
                if deps.get(k, 0) < v:
                    deps[k] = v
        waits = []
        wd = self.waited[eng]
        for k, v in deps.items():
            if eng == "pe" and k in self.ownkeys[eng]:
                continue
            if wd.get(k, 0) >= v:
                continue
            wd[k] = v
            waits.append((k, v))
        return waits

    def _mark(self, tok, reads, writes):
        k, v = tok
        self.latest[k] = v
        for b in reads:
            b.r[k] = v
        for b in writes:
            b.w = {k: v}
            b.r = {}

    def op(self, eng, fn, reads=(), writes=()):
        waits = self._deps(eng, reads, writes)
        if self.cnt[eng] >= EPOCH:
            self._new_epoch(eng)
        self.cnt[eng] += 1
        tok = (self.key[eng], self.cnt[eng])
        self.ops[eng].append((waits, fn, (tok[0], 1)))
        self._mark(tok, reads, writes)
        self.nops += 1
        return tok

    def dma(self, eng, grp, out, in_, reads=(), writes=(), **kw):
        waits = self._deps(eng, reads, writes)
        grp.cnt += 16
        tok = (grp.key, grp.cnt)
        self.ops[eng].append((waits, lambda e: e.dma_start(out=out, in_=in_, **kw), (tok[0], 16)))
        self._mark(tok, reads, writes)
        self.nops += 1
        return tok

    def wait_all(self, eng, bufs):
        waits = self._deps(eng, bufs, bufs)
        self.ops[eng].append((waits, None, None))

    def emit(self):
        nc = self.nc
        semh = self.semh
        ops = self.ops

        def replay(name, e):
            for waits, fn, inc in ops[name]:
                for k, v in waits:
                    e.wait_ge(semh[k], v)
                if fn is not None:
                    ins = fn(e)
                    ins.then_inc(semh[inc[0]], inc[1])

        with nc.Block() as block:
            @block.tensor
            def _(e):
                replay("pe", e)

            @block.vector
            def _(e):
                replay("dve", e)

            @block.scalar
            def _(e):
                replay("act", e)

            @block.gpsimd
            def _(e):
                replay("pool", e)

            @block.sync
            def _(e):
                replay("sp", e)


def _mk(P):
    def mm(out, lhsT, rhs, start, stop, reads, writes):
        return P.op("pe", lambda e: e.matmul(out, lhsT=lhsT, rhs=rhs, start=start, stop=stop), reads, writes)
    def tr(out, in_, ident, reads, writes):
        return P.op("pe", lambda e: e.transpose(out=out, in_=in_, identity=ident), reads, writes)
    def act(out, in_, func, reads, writes, **kw):
        return P.op("act", lambda e: e.activation(out=out, in_=in_, func=func, **kw), reads, writes)
    def tt(eng, out, in0, in1, op, reads, writes):
        return P.op(eng, lambda e: e.tensor_tensor(out=out, in0=in0, in1=in1, op=op), reads, writes)
    def ts(eng, out, in0, s1, s2, op0, op1, reads, writes):
        if s2 is None:
            return P.op(eng, lambda e: e.tensor_scalar(out=out, in0=in0, scalar1=s1, scalar2=None, op0=op0), reads, writes)
        return P.op(eng, lambda e: e.tensor_scalar(out=out, in0=in0, scalar1=s1, scalar2=s2, op0=op0, op1=op1), reads, writes)
    def stt(eng, out, in0, scalar, in1, op0, op1, reads, writes):
        return P.op(eng, lambda e: e.scalar_tensor_tensor(out=out, in0=in0, scalar=scalar, in1=in1, op0=op0, op1=op1), reads, writes)
    def cp(eng, out, in_, reads, writes):
        if eng == "act":
            return P.op(eng, lambda e: e.copy(out=out, in_=in_), reads, writes)
        return P.op(eng, lambda e: e.tensor_copy(out=out, in_=in_), reads, writes)
    def ms(eng, ap, val, writes):
        return P.op(eng, lambda e: e.memset(ap, val), (), writes)
    def red(eng, out, in_, op, reads, writes):
        return P.op(eng, lambda e: e.tensor_reduce(out=out, in_=in_, axis=AX.X, op=op), reads, writes)
    P.mm, P.tr, P.act, P.tt, P.ts, P.stt, P.cp, P.ms, P.red = mm, tr, act, tt, ts, stt, cp, ms, red
    return P

import math

EPS = 1e-6
PATS = ((128, 1), (512, 4), (2048, 16))


def alibi_slopes(n):
    def geometric(k):
        start = 2.0 ** (-8.0 / k)
        return [start ** (i + 1) for i in range(k)]
    p = 2 ** int(math.floor(math.log2(n)))
    s = geometric(p) + geometric(2 * p)[0::2][: n - p]
    return np.sort(np.array(s, dtype=np.float32))[::-1].copy()


def build(S=8192, debug=False, phases="ABCD", gelu_func=None):
    nc = bass.Bass("TRN2", target_bir_lowering=False)
    GELU = gelu_func or AF.Gelu_apprx_tanh
    HALF = min(S, 4096)
    NH = S // HALF
    TPH = HALF // 512
    NSB = S // 2048
    skind = "ExternalOutput" if debug else "Internal"

    def din(name, shape, dt=F32):
        return nc.dram_tensor(name, shape, dt, kind="ExternalInput").ap()

    def dscr(name, shape, dt):
        return nc.dram_tensor(name, shape, dt, kind=skind).ap()

    x = din("x", [S, 1024])
    w_in_r = din("w_in_r", [60, 128, 8, 128])
    w_out_r = din("w_out_r", [128, 8, 1024])
    w_q_r = din("w_q_r", [128, 8, 2048])
    w_pool_r = din("w_pool_r", [128, 4, 256])
    skT_in = din("skT", [128, 16, 128])
    uT_r = din("uT_r", [128, 128, 8, 128])
    v_in = din("v_in", [16384, 1024])
    vecs = din("vecs", [128, 32])
    y = nc.dram_tensor("y", [S, 1024], F32, kind="ExternalOutput").ap()

    qT = dscr("qT", [24, 128, S], BF16)
    kT = [dscr("kT%d" % p, [8, 128, S], BF16) for p in range(3)]
    g1T = dscr("g1T", [8, 128, S], BF16)
    pgT = dscr("pgT", [8, 128, S], BF16)
    vaug = dscr("vaug", [S, 2048], BF16)
    mT = dscr("mT", [8, 128, S], BF16)
    hnT_s = dscr("hnT_s", [8, 128, S], BF16)
    rT_s = dscr("rT_s", [3, 128, S], F32)
    uT_s = dscr("uT_s", [128, 128, 1024], BF16)
    v_s = dscr("v_s", [16384, 1024], BF16)

    slopes = alibi_slopes(48).reshape(3, 16)

    with ExitStack() as st:
        P = _mk(Prog(nc, st))
        mm, tr, act, tt, ts, stt, cp, ms, red = P.mm, P.tr, P.act, P.tt, P.ts, P.stt, P.cp, P.ms, P.red

        vec = P.sb("vec", [128, 32], F32); bvec = Buf()
        identf = P.sb("identf", [128, 128], F32); bidf = Buf()
        identb = P.sb("identb", [128, 128], BF16); bidb = Buf()
        bd = P.sb("bd", [128, 128], BF16); bbd = Buf()
        io128 = P.sb("io128", [128, 128], F32); bio = Buf()
        gq = P.sb("gq", [128, 2], F32); bgq = Buf()
        epsc = P.sb("epsc", [128, 1], F32); beps = Buf()
        P.op("pool", lambda e: e.memset(epsc[:], EPS), (), [beps])
        gc = P.grp("c")
        P.dma("sp", gc, vec[:], vecs, writes=[bvec])
        ms("pool", identf[:], 0.0, [bidf])
        P.op("pool", lambda e: e.affine_select(out=identf[:], in_=identf[:], pattern=[[-1, 128]], compare_op=ALU.not_equal,
                                               fill=1.0, base=0, channel_multiplier=1), [bidf], [bidf])
        cp("pool", identb[:], identf[:], [bidf], [bidb])
        ms("pool", bd[:], 0.0, [bbd])
        ms("pool", bd[0:64, 0:64], 1.0 / 64, [bbd])
        ms("pool", bd[64:128, 64:128], 1.0 / 64, [bbd])
        P.op("pool", lambda e: e.iota(io128[:], pattern=[[1, 128]], base=0, channel_multiplier=0,
                                      allow_small_or_imprecise_dtypes=True), (), [bio])
        ts("dve", gq[:, 0:1], vec[:, 24:25], 0.125, None, ALU.mult, None, [bvec], [bgq])
        cp("dve", gq[:, 1:2], vec[:, 25:26], [bvec, bgq], [bgq])
        g1col = vec[:, 0:8]; g2col = vec[:, 8:16]; pscol = vec[:, 16:24]

        PS = [P.ps("ps%d" % i, [128, 512], F32) for i in range(8)]
        PSB = [Buf("ps%d" % i) for i in range(8)]

        def norm_transpose(src_ap, bsrc, ssq, bssq, junk, bjunk, rstd, brstd, xs, bxs, ptr_bank, gcol, dst_ap, bdst):
            ms("pool", ssq, 0.0, [bssq])
            act(junk, src_ap, AF.Square, [bsrc, bssq], [bjunk, bssq], accum_out=ssq)
            act(rstd, ssq, AF.Sqrt, [bssq, beps], [brstd], scale=1.0 / 1024, bias=epsc[:])
            P.op("dve", lambda e: e.reciprocal(out=rstd, in_=rstd), [brstd], [brstd])
            act(xs, src_ap, AF.Copy, [bsrc, brstd], [bxs], scale=rstd)
            pt = PS[ptr_bank].bitcast(BF16)
            for c in range(8):
                tr(pt[:, c * 128:(c + 1) * 128], xs[:, c * 128:(c + 1) * 128], identb[:], [bxs, bidb], [PSB[ptr_bank]])
            tt("dve", dst_ap, pt[:, 0:1024].rearrange("p (c t) -> p c t", c=8),
               gcol.unsqueeze(2).to_broadcast([128, 8, 128]), ALU.mult, [PSB[ptr_bank], bvec], [bdst])

        if "A" in phases:
            with ExitStack() as sa:
                def sb(name, shape, dt):
                    return sa.enter_context(nc.sbuf_tensor("a_" + name, shape, dt))
                xnT = sb("xnT", [128, 8, HALF], BF16); bxn = Buf()
                xts = [sb("xt%d" % i, [128, 1024], F32) for i in range(2)]; bxt = [Buf(), Buf()]; gxt = [P.grp("x"), P.grp("x")]
                junk = sb("junk", [128, 1024], F32); bjunk = Buf()
                ssq = sb("ssq", [128, 1], F32); bssq = Buf()
                rstd1 = sb("rstd1", [128, 1], F32); brstd1 = Buf()
                xs = sb("xs", [128, 1024], BF16); bxs = Buf()
                wst = [sb("wst%d" % i, [128, 8, 128], F32) for i in range(2)]; bwst = [Buf(), Buf()]; gwst = [P.grp("w"), P.grp("w")]
                wbf = [sb("wbf%d" % i, [128, 8, 128], BF16) for i in range(2)]; bwbf = [Buf(), Buf()]
                wv = sb("wv", [128, 8, 1024], BF16); bwv = Buf()
                wpst = sb("wpst", [128, 4, 256], F32); bwpst = Buf()
                wpb = sb("wpb", [128, 4, 256], BF16); bwpb = Buf()
                NST = 4
                stg = [sb("stg%d" % i, [128, 2048], BF16) for i in range(NST)]; bstg = [Buf() for _ in range(NST)]
                gstg = [P.grp("st") for _ in range(NST)]
                kst = [sb("kst%d" % i, [128, 2048], BF16) for i in range(2)]; bkst = [Buf(), Buf()]; gkst = [P.grp("k"), P.grp("k")]
                sq = [sb("sq%d" % i, [128, 512], BF16) for i in range(2)]; bsq = [Buf(), Buf()]
                rs = [sb("rs%d" % i, [128, 512], F32) for i in range(2)]; brs = [Buf(), Buf()]
                abuf = sb("abuf", [128, 528], F32); bab = Buf()
                sA = sb("sA", [128, 528], F32); bsA = Buf()
                sB = sb("sB", [128, 528], F32); bsB = Buf()
                dT = sb("dT", [128, HALF], BF16); bdT = Buf()
                ahalo = sb("ahalo", [128, 4, 16], F32); bah = Buf()
                rc = sb("rc", [128, 4, 16], F32); brc = Buf()
                tmp16 = sb("tmp16", [128, 16], F32); btmp16 = Buf()
                g0s = [sb("g0s%d" % i, [128, 512], BF16) for i in range(2)]; bg0s = [Buf(), Buf()]
                vst = [sb("vst%d" % i, [128, 2048], BF16) for i in range(2)]; bvst = [Buf(), Buf()]; gvst = [P.grp("v"), P.grp("v")]
                cst = [sb("cst%d" % i, [128, 2048], F32) for i in range(2)]; bcst = [Buf(), Buf()]; gcst = [P.grp("cs"), P.grp("cs")]
                cbf = [sb("cbf%d" % i, [128, 2048], BF16) for i in range(2)]; bcbf = [Buf(), Buf()]; gcbf = [P.grp("cb"), P.grp("cb")]

                gA = P.grp("A")
                P.dma("sp", gA, wpst[:], w_pool_r, writes=[bwpst])
                cp("pool", wpb[:], wpst[:], [bwpst], [bwpb])
                ms("pool", ahalo[:], 0.0, [bah])
                for g in range(4):
                    w = PATS and (2, 4, 8, 16)[g]
                    ts("pool", rc[:, g, :], io128[:, 0:16], 1.0, float(w), ALU.add, ALU.min, [bio], [brc])
                P.op("dve", lambda e: e.reciprocal(out=rc[:], in_=rc[:]), [brc], [brc])
                for i in range(2):
                    ms("pool", vst[i][:], 1.0, [bvst[i]])

                p0_items = []
                if "D" in phases:
                    for it in range(64):
                        p0_items.append(("u", it))
                        p0_items.append(("v", it))
                p0_state = {"i": 0}

                def p0_step():
                    i = p0_state["i"]
                    if i >= len(p0_items):
                        return
                    p0_state["i"] = i + 1
                    kind, it = p0_items[i]
                    s = i % 2
                    if kind == "u":
                        src = uT_r[it * 2:(it + 1) * 2].rearrange("j p c n -> p j (c n)")
                        dst = uT_s[it * 2:(it + 1) * 2].rearrange("j p f -> p j f")
                    else:
                        src = v_in[it * 256:(it + 1) * 256, :].rearrange("(j n) f -> n j f", n=128)
                        dst = v_s[it * 256:(it + 1) * 256, :].rearrange("(j n) f -> n j f", n=128)
                    P.dma("sp", gcst[s], cst[s][:].rearrange("p (j f) -> p j f", j=2), src, writes=[bcst[s]])
                    cp("pool" if (i % 4) < 2 else "act", cbf[s][:], cst[s][:], [bcst[s]], [bcbf[s]])
                    P.dma("pool", gcbf[s], dst, cbf[s][:].rearrange("p (j f) -> p j f", j=2), reads=[bcbf[s]])

                mmcnt = [0]
                stgcnt = [0]
                pendA = [None]
                for half in range(NH):
                    h0 = half * HALF
                    for b in range(HALF // 128):
                        s = b % 2
                        t0 = h0 + b * 128
                        P.dma("sp", gxt[s], xts[s][:], x[t0:t0 + 128, :], writes=[bxt[s]])
                        norm_transpose(xts[s][:], bxt[s], ssq[:], bssq, junk[:], bjunk, rstd1[:], brstd1, xs[:], bxs, 7, g1col,
                                       xnT[:, :, b * 128:(b + 1) * 128], bxn)
                    for k in range(8):
                        s = k % 2
                        P.dma("sp", gwst[s], wst[s][:], w_in_r[36 + k], writes=[bwst[s]])
                        cp("pool", wv[:, :, k * 128:(k + 1) * 128], wst[s][:], [bwst[s]], [bwv])
                    for b in range(HALF // 128):
                        s = b % 2
                        t0 = h0 + b * 128
                        vv = vst[s][:].rearrange("p (q j e) -> p q j e", q=8, j=2)
                        for hf in range(2):
                            bank = 5 + hf
                            for c in range(8):
                                mm(PS[bank][:, :], xnT[:, c, b * 128:(b + 1) * 128], wv[:, c, hf * 512:(hf + 1) * 512],
                                   c == 0, c == 7, [bxn, bwv], [PSB[bank]])
                            pv = PS[bank][:, :].rearrange("p (q j e) -> p q j e", q=4, j=2)
                            act(vv[:, hf * 4:(hf + 1) * 4, 0, 0:64], pv[:, :, 0, :], AF.Copy, [PSB[bank]], [bvst[s]])
                            act(vv[:, hf * 4:(hf + 1) * 4, 1, 64:128], pv[:, :, 1, :], AF.Copy, [PSB[bank]], [bvst[s]])
                        P.dma("pool", gvst[s], vaug[t0:t0 + 128, :], vst[s][:], reads=[bvst[s]])

                    order = []
                    for g in range(4):
                        order.append(("a", g, g))
                        for oc in range(2):
                            order.append(("y", 44 + g * 2 + oc, g * 2 + oc))
                    for qc in range(24):
                        order.append(("q", 4 + qc, qc))
                    for kc in range(8):
                        order.append(("k", 28 + kc, kc))
                    for c in range(8):
                        order.append(("g1", 52 + c, c))
                    def load_w(oi_):
                        ws_ = oi_ % 2
                        P.dma("sp", gwst[ws_], wst[ws_][:], w_in_r[order[oi_][1]], writes=[bwst[ws_]])
                        cp("pool", wbf[ws_][:], wst[ws_][:], [bwst[ws_]], [bwbf[ws_]])
                    load_w(0)
                    for oi, (typ, fc, idx) in enumerate(order):
                        ws = oi % 2
                        if oi + 1 < len(order):
                            load_w(oi + 1)
                        p0_step()
                        for tti in range(TPH):
                            bank = (0, 1, 4, 5, 6)[mmcnt[0] % 5]
                            mmcnt[0] += 1
                            tsl = slice(tti * 512, (tti + 1) * 512)
                            for c in range(8):
                                mm(PS[bank][:, :], wbf[ws][:, c, :], xnT[:, c, tsl], c == 0, c == 7, [bwbf[ws], bxn], [PSB[bank]])
                            def post_tile(typ=typ, idx=idx, tti=tti, bank=bank, half=half, h0=h0, tsl=tsl):
                                pp = PS[bank][:, :]
                                tl = tti % 4
                                sbi = (h0 // 2048) + tti // 4
                                if typ == "a":
                                    g = idx
                                    wsz = (2, 4, 8, 16)[g]
                                    if tti == 0:
                                        cp("pool", abuf[:, 0:16], ahalo[:, g, :], [bah], [bab])
                                    act(abuf[:, 16:528], pp, AF.Copy, [PSB[bank]], [bab])
                                    cur, bcur = abuf, bab
                                    srcs = [(sA, bsA), (sB, bsB)]
                                    off = 0
                                    for stp in range(g + 1):
                                        sh = 1 << stp
                                        off += sh
                                        dst, bdst = srcs[stp % 2]
                                        tt("pool", dst[:, off:528], cur[:, off:528], cur[:, off - sh:528 - sh], ALU.add, [bcur], [bdst])
                                        cur, bcur = dst, bdst
                                    stt("dve", dT[:, tsl], cur[:, 16:528], 1.0 / wsz, abuf[:, 16:528], ALU.mult, ALU.subtract,
                                        [bcur, bab], [bdT])
                                    if half == 0 and tti == 0:
                                        tt("pool", tmp16[:], cur[:, 16:32], rc[:, g, :], ALU.mult, [bcur, brc], [btmp16])
                                        tt("pool", dT[:, 0:16], tmp16[:], abuf[:, 16:32], ALU.subtract, [btmp16, bab], [bdT])
                                    if tti == TPH - 1:
                                        cp("pool", ahalo[:, g, :], abuf[:, 512:528], [bab], [bah])
                                    else:
                                        cp("pool", abuf[:, 0:16], abuf[:, 512:528], [bab], [bab])
                                elif typ == "y":
                                    yc = idx
                                    g, oc = yc // 2, yc % 2
                                    gs = tti % 2
                                    act(g0s[gs][:], pp, AF.Sigmoid, [PSB[bank]], [bg0s[gs]])
                                    ybank = 2 + (tti % 2)
                                    mm(PS[ybank][:, :], wpb[:, g, oc * 128:(oc + 1) * 128], dT[:, tsl], True, True, [bwpb, bdT], [PSB[ybank]])
                                    si = stgcnt[0] % NST
                                    stt("dve", stg[si][:, tl * 512:(tl + 1) * 512], PS[ybank][:, :], pscol[:, yc:yc + 1], g0s[gs][:],
                                        ALU.mult, ALU.mult, [PSB[ybank], bg0s[gs], bvec], [bstg[si]])
                                    if tl == 3:
                                        P.dma("pool", gstg[si], pgT[yc, :, sbi * 2048:(sbi + 1) * 2048], stg[si][:], reads=[bstg[si]])
                                        stgcnt[0] += 1
                                elif typ in ("q", "k"):
                                    ss_ = tti % 2
                                    act(sq[ss_][:], pp, AF.Square, [PSB[bank]], [bsq[ss_]])
                                    sbank = 2 + (tti % 2)
                                    mm(PS[sbank][:, :], bd[:], sq[ss_][:], True, True, [bbd, bsq[ss_]], [PSB[sbank]])
                                    act(rs[ss_][:], PS[sbank][:, :], AF.Sqrt, [PSB[sbank], beps], [brs[ss_]], bias=epsc[:])
                                    P.op("dve", (lambda t: (lambda e: e.reciprocal(out=t, in_=t)))(rs[ss_][:]), [brs[ss_]], [brs[ss_]])
                                    gcol = gq[:, 0:1] if typ == "q" else gq[:, 1:2]
                                    if typ == "q":
                                        p = idx // 8
                                        d = PATS[p][1]
                                        si = stgcnt[0] % NST
                                        if d == 1:
                                            o_ap = stg[si][:, tl * 512:(tl + 1) * 512]
                                            i0_ap, i1_ap = pp, rs[ss_][:]
                                        elif d == 4:
                                            o_ap = stg[si][:, tl * 512:(tl + 1) * 512].rearrange("p (r i) -> p i r", r=4)
                                            i0_ap = pp.rearrange("p (i r) -> p i r", r=4)
                                            i1_ap = rs[ss_][:].rearrange("p (i r) -> p i r", r=4)
                                        else:
                                            o_ap = stg[si][:, :].rearrange("p (r i) -> p i r", r=16)[:, tl * 32:(tl + 1) * 32, :]
                                            i0_ap = pp.rearrange("p (i r) -> p i r", r=16)
                                            i1_ap = rs[ss_][:].rearrange("p (i r) -> p i r", r=16)
                                        stt("dve", o_ap, i0_ap, gcol, i1_ap, ALU.mult, ALU.mult, [PSB[bank], brs[ss_], bgq], [bstg[si]])
                                        if tl == 3:
                                            P.dma("pool", gstg[si], qT[idx, :, sbi * 2048:(sbi + 1) * 2048], stg[si][:], reads=[bstg[si]])
                                            stgcnt[0] += 1
                                    else:
                                        si = stgcnt[0] % NST
                                        nat = stg[si][:, tl * 512:(tl + 1) * 512]
                                        stt("dve", nat, pp, gcol, rs[ss_][:], ALU.mult, ALU.mult, [PSB[bank], brs[ss_], bgq], [bstg[si]])
                                        cp("pool", kst[0][:, tl * 512:(tl + 1) * 512].rearrange("p (r i) -> p i r", r=4),
                                           nat.rearrange("p (i r) -> p i r", r=4), [bstg[si]], [bkst[0]])
                                        cp("pool", kst[1][:, :].rearrange("p (r i) -> p i r", r=16)[:, tl * 32:(tl + 1) * 32, :],
                                           nat.rearrange("p (i r) -> p i r", r=16), [bstg[si]], [bkst[1]])
                                        if tl == 3:
                                            ssl = slice(sbi * 2048, (sbi + 1) * 2048)
                                            P.dma("pool", gstg[si], kT[0][idx, :, ssl], stg[si][:], reads=[bstg[si]])
                                            P.dma("pool", gkst[0], kT[1][idx, :, ssl], kst[0][:], reads=[bkst[0]])
                                            P.dma("pool", gkst[1], kT[2][idx, :, ssl], kst[1][:], reads=[bkst[1]])
                                            stgcnt[0] += 1
                                else:
                                    si = stgcnt[0] % NST
                                    act(stg[si][:, tl * 512:(tl + 1) * 512], pp, AF.Sigmoid, [PSB[bank]], [bstg[si]])
                                    if tl == 3:
                                        P.dma("pool", gstg[si], g1T[idx, :, sbi * 2048:(sbi + 1) * 2048], stg[si][:], reads=[bstg[si]])
                                        stgcnt[0] += 1
                            if pendA[0] is not None:
                                pendA[0]()
                            pendA[0] = post_tile
                    if pendA[0] is not None:
                        pendA[0]()
                        pendA[0] = None
                while p0_state["i"] < len(p0_items):
                    p0_step()
                P.barrier()

        if "B" in phases:
            with ExitStack() as sa:
                def sb(name, shape, dt):
                    return sa.enter_context(nc.sbuf_tensor("b_" + name, shape, dt))
                M = sb("M", [128, 48, 256], BF16); bM = Buf()
                tq = sb("tq", [128, 128], F32); btq = Buf()
                tA = sb("tA", [128, 128], F32); btA = Buf()
                tB = sb("tB", [128, 128], F32); btB = Buf()
                tC = sb("tC", [128, 128], F32); btC = Buf()
                P.op("pool", lambda e: e.iota(tq[:], pattern=[[1, 128]], base=0, channel_multiplier=-1,
                                              allow_small_or_imprecise_dtypes=True), (), [btq])
                for p in range(3):
                    d = PATS[p][1]
                    for h in range(16):
                        c = float(slopes[p, h]) * d
                        ph = p * 16 + h
                        ts("pool", tA[:], tq[:], 0.0, None, ALU.max, None, [btq], [btA])
                        act(tB[:], tA[:], AF.Exp, [btA], [btB], scale=-c)
                        P.op("pool", lambda e: e.affine_select(out=tC[:], in_=tB[:], pattern=[[1, 128]], compare_op=ALU.is_ge,
                                                               fill=0.0, base=0, channel_multiplier=-1), [btB], [btC])
                        cp("dve", M[:, ph, 128:256], tC[:], [btC], [bM])
                        ts("pool", tA[:], tq[:], 0.0, 128.0, ALU.min, ALU.add, [btq], [btA])
                        act(tB[:], tA[:], AF.Exp, [btA], [btB], scale=-c)
                        P.op("pool", lambda e: e.affine_select(out=tC[:], in_=tB[:], pattern=[[-1, 128]], compare_op=ALU.is_ge,
                                                               fill=0.0, base=0, channel_multiplier=1), [btB], [btC])
                        cp("dve", M[:, ph, 0:128], tC[:], [btC], [bM])
                q3 = sb("q3", [128, 3, 2048], BF16); bq3 = Buf(); gq3 = P.grp("q3")
                k3 = [sb("k3_%d" % i, [128, 3, 2048], BF16) for i in range(2)]; bk3 = [Buf(), Buf()]; gk3 = [P.grp("k3"), P.grp("k3")]
                va = [sb("va_%d" % i, [128, 3, 16, 256], BF16) for i in range(2)]; bva = [Buf(), Buf()]; gva = [P.grp("va"), P.grp("va")]
                g1c = sb("g1c", [128, 2048], BF16); bg1c = Buf(); gg1 = P.grp("g1c")
                pgc = sb("pgc", [128, 2048], BF16); bpgc = Buf(); gpg = P.grp("pgc")
                Oacc = [sb("Oacc%d" % i, [128, 2048], F32) for i in range(2)]; bOa = [Buf(), Buf()]
                att = sb("att", [128, 2048], F32); batt = Buf()
                rd = sb("rd", [128, 2048], F32); brd = Buf()
                mc = sb("mc", [128, 2048], BF16); bmc = Buf(); gmc = P.grp("mc")
                NE = 4
                E = [sb("E%d" % i, [128, 512], F32) for i in range(NE)]; bE = [Buf() for _ in range(NE)]
                PT = [sb("PT%d" % i, [128, 512], BF16) for i in range(NE)]; bPT = [Buf() for _ in range(NE)]
                ecnt = [0]
                ocnt = [0]

                def vrecip_b(out, in_, reads, writes):
                    return P.op("dve", lambda e: e.reciprocal(out=out, in_=in_), reads, writes)
                for hp in range(8):
                    for sbi in range(NSB):
                        cur = sbi % 2
                        prv = 1 - cur
                        ssl = slice(sbi * 2048, (sbi + 1) * 2048)
                        for p in range(3):
                            P.dma("sp", gq3, q3[:, p, :], qT[p * 8 + hp, :, ssl], writes=[bq3])
                            P.dma("sp", gk3[cur], k3[cur][:, p, :], kT[p][hp, :, ssl], writes=[bk3[cur]])
                            d = PATS[p][1]
                            nb = 16 // d
                            if d == 1:
                                src = vaug[sbi * 2048:(sbi + 1) * 2048, hp * 256:(hp + 1) * 256].rearrange("(n i) f -> i n f", i=128)
                                P.dma("sp", gva[cur], va[cur][:, p, :, :], src, writes=[bva[cur]])
                            else:
                                for n in range(nb):
                                    r0 = sbi * 2048 + n * 128 * d
                                    src = vaug[r0:r0 + 128 * d, hp * 256:(hp + 1) * 256].rearrange("(i r) f -> i r f", r=d)
                                    P.dma("sp", gva[cur], va[cur][:, p, n * d:(n + 1) * d, :], src, writes=[bva[cur]])
                        P.dma("sp", gg1, g1c[:], g1T[hp, :, ssl], writes=[bg1c])
                        P.dma("sp", gpg, pgc[:], pgT[hp, :, ssl], writes=[bpgc])
                        def make_item(hh, p, grp4, cp2, obank, sbank, ei, cur, prv, sbi, hp):
                            h = hp * 2 + hh
                            rows = slice(hh * 64, hh * 64 + 64)
                            d = PATS[p][1]
                            ph = p * 16 + h
                            pS = PS[sbank][:, :].rearrange("p (c j q) -> p c j q", c=2, j=2)
                            Ev = E[ei][:].rearrange("p (c j q) -> p c j q", c=2, j=2)
                            PTv = PT[ei][:].rearrange("p (c j q) -> p c j q", c=2, j=2)
                            infos = []
                            for j in range(2):
                                cl = grp4 * 4 + cp2 * 2 + j
                                if cl >= d:
                                    infos.append((cl, k3[cur][rows, p, (cl - d) * 128:(cl - d + 1) * 128],
                                                  va[cur][:, p, cl - d, hh * 128:(hh + 1) * 128], bk3[cur], bva[cur]))
                                elif sbi > 0:
                                    infos.append((cl, k3[prv][rows, p, (cl + 16 - d) * 128:(cl + 17 - d) * 128],
                                                  va[prv][:, p, cl + 16 - d, hh * 128:(hh + 1) * 128], bk3[prv], bva[prv]))
                                else:
                                    infos.append((cl, None, None, None, None))

                            def qk():
                                for j in range(2):
                                    cl, kp_ap, vp_ap, bkp, bvp = infos[j]
                                    qap = q3[rows, p, cl * 128:(cl + 1) * 128]
                                    kc_ap = k3[cur][rows, p, cl * 128:(cl + 1) * 128]
                                    mm(pS[:, j, 1, :], kc_ap, qap, True, True, [bk3[cur], bq3], [PSB[sbank]])
                                    if kp_ap is not None:
                                        mm(pS[:, j, 0, :], kp_ap, qap, True, True, [bkp, bq3], [PSB[sbank]])

                            def post():
                                allprev = all(i[1] is not None for i in infos)
                                if allprev:
                                    act(E[ei][:], PS[sbank][:, :], AF.Exp, [PSB[sbank]], [bE[ei]])
                                    tt("dve", PTv, Ev, M[:, ph, :].rearrange("p (j q) -> p j q", j=2).unsqueeze(1).to_broadcast([128, 2, 2, 128]),
                                       ALU.mult, [bE[ei], bM], [bPT[ei]])
                                else:
                                    act(Ev[:, :, 1, :], pS[:, :, 1, :], AF.Exp, [PSB[sbank]], [bE[ei]])
                                    tt("dve", PTv[:, :, 1, :], Ev[:, :, 1, :], M[:, ph, 128:256].unsqueeze(1).to_broadcast([128, 2, 128]),
                                       ALU.mult, [bE[ei], bM], [bPT[ei]])
                                    for j in range(2):
                                        if infos[j][1] is not None:
                                            act(Ev[:, j, 0, :], pS[:, j, 0, :], AF.Exp, [PSB[sbank]], [bE[ei]])
                                            tt("dve", PTv[:, j, 0, :], Ev[:, j, 0, :], M[:, ph, 0:128], ALU.mult, [bE[ei], bM], [bPT[ei]])

                            def pv():
                                pO = PS[obank][:, :].rearrange("p (c q) -> p c q", c=4)
                                for j in range(2):
                                    cl, kp_ap, vp_ap, bkp, bvp = infos[j]
                                    oj = cp2 * 2 + j
                                    vc_ap = va[cur][:, p, cl, hh * 128:(hh + 1) * 128]
                                    if vp_ap is not None:
                                        mm(pO[:, oj, :], vp_ap, PTv[:, j, 0, :], True, False, [bvp, bPT[ei]], [PSB[obank]])
                                        mm(pO[:, oj, :], vc_ap, PTv[:, j, 1, :], False, True, [bva[cur], bPT[ei]], [PSB[obank]])
                                    else:
                                        mm(pO[:, oj, :], vc_ap, PTv[:, j, 1, :], True, True, [bva[cur], bPT[ei]], [PSB[obank]])
                                if cp2 == 1:
                                    if d == 1:
                                        oap = Oacc[hh][:, grp4 * 512:(grp4 + 1) * 512].rearrange("p (c q) -> p c q", c=4)
                                    elif d == 4:
                                        oap = Oacc[hh][:, grp4 * 512:(grp4 + 1) * 512].rearrange("p (i r) -> p r i", r=4)
                                    else:
                                        oap = Oacc[hh][:, :].rearrange("p (i r) -> p r i", r=16)[:, grp4 * 4:(grp4 + 1) * 4, :]
                                    if p == 0:
                                        cp("dve", oap, pO, [PSB[obank]], [bOa[hh]])
                                    else:
                                        tt("dve", oap, oap, pO, ALU.add, [PSB[obank], bOa[hh]], [bOa[hh]])
                                if p == 2 and grp4 == 3 and cp2 == 1:
                                    if hh == 0:
                                        num, den = Oacc[0][0:64, :], Oacc[0][64:128, :]
                                    else:
                                        num, den = Oacc[1][64:128, :], Oacc[1][0:64, :]
                                    vrecip_b(rd[rows, :], den, [bOa[hh]], [brd])
                                    tt("dve", att[rows, :], num, rd[rows, :], ALU.mult, [bOa[hh], brd], [batt])
                                    tt("pool", att[rows, :], att[rows, :], g1c[rows, :], ALU.mult, [batt, bg1c], [batt])
                                    tt("pool", mc[rows, :], att[rows, :], pgc[rows, :], ALU.add, [batt, bpgc], [bmc])
                            return qk, post, pv

                        items = []
                        for hh in range(2):
                            for p in range(3):
                                for grp4 in range(4):
                                    obank = (3, 4, 6)[ocnt[0] % 3]
                                    ocnt[0] += 1
                                    for cp2 in range(2):
                                        sbank = (0, 1, 2, 5)[ecnt[0] % 4]
                                        ei = ecnt[0] % NE
                                        ecnt[0] += 1
                                        items.append(make_item(hh, p, grp4, cp2, obank, sbank, ei, cur, prv, sbi, hp))
                        for ii, (qk, post, pv) in enumerate(items):
                            qk()
                            post()
                            if ii > 0:
                                items[ii - 1][2]()
                        items[-1][2]()
                        P.dma("pool", gmc, mT[hp, :, ssl], mc[:], reads=[bmc])
                P.barrier()

        def vmax(out, in_, reads, writes):
            return P.op("dve", lambda e: e.max(out=out, in_=in_), reads, writes)

        def vmaxidx(out, in_max, in_values, reads, writes):
            return P.op("dve", lambda e: e.max_index(out=out, in_max=in_max, in_values=in_values), reads, writes)

        def vmr(out, rep, vals, reads, writes):
            return P.op("dve", lambda e: e.match_replace(out=out, in_to_replace=rep, in_values=vals, imm_value=-1e30), reads, writes)

        def vrecip(out, in_, reads, writes):
            return P.op("dve", lambda e: e.reciprocal(out=out, in_=in_), reads, writes)

        if "C" in phases:
            with ExitStack() as sa:
                def sb(name, shape, dt):
                    return sa.enter_context(nc.sbuf_tensor("c_" + name, shape, dt))
                woutb = sb("woutb", [128, 8, 1024], BF16); bwo = Buf()
                wqb = sb("wqb", [128, 8, 2048], BF16); bwq = Buf()
                skb = sb("skb", [128, 16, 128], BF16); bsk = Buf()
                wl = [sb("wl%d" % i, [128, 2048], F32) for i in range(2)]; bwl = [Buf(), Buf()]; gwl = [P.grp("wl"), P.grp("wl")]
                pieces = []
                for i in range(4):
                    pieces.append((w_out_r[:, 2 * i:2 * i + 2, :].rearrange("p c f -> p (c f)"),
                                   woutb[:, 2 * i:2 * i + 2, :].rearrange("p c f -> p (c f)"), bwo))
                for i in range(8):
                    pieces.append((w_q_r[:, i, :], wqb[:, i, :], bwq))
                pieces.append((skT_in.rearrange("p h n -> p (h n)"), skb[:].rearrange("p h n -> p (h n)"), bsk))
                for i, (src, dst, bd_) in enumerate(pieces):
                    s = i % 2
                    P.dma("sp", gwl[s], wl[s][:], src, writes=[bwl[s]])
                    cp("pool" if i % 2 else "act", dst, wl[s][:], [bwl[s]], [bd_])
                mt = [sb("mt%d" % i, [128, 8, 512], BF16) for i in range(2)]; bmt = [Buf(), Buf()]; gmt = [P.grp("mt"), P.grp("mt")]
                xt2 = [sb("xt2_%d" % i, [128, 1024], F32) for i in range(2)]; bxt2 = [Buf(), Buf()]; gxt2 = [P.grp("x2"), P.grp("x2")]
                ht = [sb("ht%d" % i, [128, 1024], F32) for i in range(2)]; bht = [Buf(), Buf()]; ght = [P.grp("ht"), P.grp("ht")]
                junk = sb("junk", [128, 1024], F32); bjunk = Buf()
                ssq = sb("ssq", [128, 1], F32); bssq = Buf()
                rstd1 = sb("rstd1", [128, 1], F32); brstd1 = Buf()
                xs = sb("xs", [128, 1024], BF16); bxs = Buf()
                hnst = [sb("hnst%d" % i, [128, 8, 512], BF16) for i in range(2)]; bhn = [Buf(), Buf()]; ghn = [P.grp("hn"), P.grp("hn")]
                rtst = [sb("rtst%d" % i, [128, 3, 512], F32) for i in range(2)]; brt = [Buf(), Buf()]; grt = [P.grp("rt"), P.grp("rt")]
                qpT = sb("qpT", [128, 16, 128], BF16); bqp = Buf()
                sc = sb("sc", [128, 16, 128], F32); bsc = Buf()
                sc2 = sb("sc2", [128, 16, 128], F32); bsc2 = Buf()
                ts1 = sb("ts1", [128, 16, 16], F32); bts1 = Buf()
                ti1 = sb("ti1", [128, 16, 16], U32); bti1 = Buf()
                tif = sb("tif", [128, 16, 16], F32); btif = Buf()
                cand = sb("cand", [128, 8, 256], F32); bcand = Buf()
                cand2 = sb("cand2", [128, 8, 256], F32); bcand2 = Buf()
                bs = sb("bs", [128, 8, 16], F32); bbs = Buf()
                bj = sb("bj", [128, 8, 16], U32); bbj = Buf()
                bjf = sb("bjf", [128, 8, 16], F32); bbjf = Buf()
                ba = sb("ba", [128, 8, 16], F32); bba = Buf()
                bb_ = sb("bb", [128, 8, 16], F32); bbb = Buf()
                oh = sb("oh", [128, 8, 16, 16], F32); boh = Buf()
                prod = sb("prod", [128, 8, 16, 16], F32); bprod = Buf()
                ri = sb("ri", [128, 3, 128], F32); bri = Buf()
                ex = sb("ex", [128, 8, 16], F32); bex = Buf()
                sm = sb("sm", [128, 8], F32); bsm = Buf()
                io16b = io128[:, 0:16].unsqueeze(1).unsqueeze(1).to_broadcast([128, 8, 16, 16])
                thr = sb("thr", [128, 16], F32); bthr = Buf()
                P.op("pool", lambda e: e.iota(thr[:], pattern=[[16, 16]], base=16, channel_multiplier=0,
                                              allow_small_or_imprecise_dtypes=True), (), [bthr])
                thrb = thr[:].unsqueeze(1).unsqueeze(1).to_broadcast([128, 8, 16, 16])
                for grp_i in range(S // 512):
                    gs = grp_i % 2
                    tg0 = grp_i * 512
                    P.dma("sp", gmt[gs], mt[gs][:], mT[:, :, tg0:tg0 + 512].rearrange("c p t -> p c t"), writes=[bmt[gs]])
                    for b in range(4):
                        blk = grp_i * 4 + b
                        s = blk % 2
                        t0 = tg0 + b * 128
                        bsl = slice(b * 128, (b + 1) * 128)
                        P.dma("sp", gxt2[s], xt2[s][:], x[t0:t0 + 128, :], writes=[bxt2[s]])
                        for hf in range(2):
                            for c in range(8):
                                mm(PS[hf][:, :], mt[gs][:, c, bsl], woutb[:, c, hf * 512:(hf + 1) * 512], c == 0, c == 7,
                                   [bmt[gs], bwo], [PSB[hf]])
                        for hf in range(2):
                            tt("dve", ht[s][:, hf * 512:(hf + 1) * 512], PS[hf][:, :], xt2[s][:, hf * 512:(hf + 1) * 512], ALU.add,
                               [PSB[hf], bxt2[s]], [bht[s]])
                        P.dma("pool", ght[s], y[t0:t0 + 128, :], ht[s][:], reads=[bht[s]])
                        norm_transpose(ht[s][:], bht[s], ssq[:], bssq, junk[:], bjunk, rstd1[:], brstd1, xs[:], bxs, 7, g2col,
                                       hnst[gs][:, :, bsl], bhn[gs])
                        for j in range(16):
                            bank = 2 + j // 4
                            for c in range(8):
                                mm(PS[bank][:, (j % 4) * 128:(j % 4 + 1) * 128], wqb[:, c, j * 128:(j + 1) * 128], hnst[gs][:, c, bsl],
                                   c == 0, c == 7, [bwq, bhn[gs]], [PSB[bank]])
                        for g4 in range(4):
                            act(qpT[:, g4 * 4:(g4 + 1) * 4, :].rearrange("p j t -> p (j t)"), PS[2 + g4][:, :], AF.Copy, [PSB[2 + g4]], [bqp])
                        for j in range(16):
                            bank = 2 + j // 4
                            mm(PS[bank][:, (j % 4) * 128:(j % 4 + 1) * 128], qpT[:, j, :], skb[:, j, :], True, True, [bqp, bsk], [PSB[bank]])
                        for g4 in range(4):
                            act(sc[:, g4 * 4:(g4 + 1) * 4, :].rearrange("p j t -> p (j t)"), PS[2 + g4][:, :], AF.Copy, [PSB[2 + g4]], [bsc])
                        for j in range(16):
                            vmax(ts1[:, j, 0:8], sc[:, j, :], [bsc], [bts1])
                            vmaxidx(ti1[:, j, 0:8], ts1[:, j, 0:8], sc[:, j, :], [bsc, bts1], [bti1])
                            vmr(sc2[:, j, :], ts1[:, j, 0:8], sc[:, j, :], [bsc, bts1], [bsc2])
                            vmax(ts1[:, j, 8:16], sc2[:, j, :], [bsc2], [bts1])
                            vmaxidx(ti1[:, j, 8:16], ts1[:, j, 8:16], sc2[:, j, :], [bsc2, bts1], [bti1])
                        cp("dve", tif[:], ti1[:], [bti1], [btif])
                        ts1v = ts1[:].rearrange("p (h t) k -> p h t k", t=2)
                        tifv = tif[:].rearrange("p (h t) k -> p h t k", t=2)
                        tt("dve", cand[:].rearrange("p h (a b) -> p h a b", a=16),
                           ts1v[:, :, 0, :].unsqueeze(3).to_broadcast([128, 8, 16, 16]),
                           ts1v[:, :, 1, :].unsqueeze(2).to_broadcast([128, 8, 16, 16]), ALU.add, [bts1], [bcand])
                        for h in range(8):
                            vmax(bs[:, h, 0:8], cand[:, h, :], [bcand], [bbs])
                            vmaxidx(bj[:, h, 0:8], bs[:, h, 0:8], cand[:, h, :], [bcand, bbs], [bbj])
                            vmr(cand2[:, h, :], bs[:, h, 0:8], cand[:, h, :], [bcand, bbs], [bcand2])
                            vmax(bs[:, h, 8:16], cand2[:, h, :], [bcand2], [bbs])
                            vmaxidx(bj[:, h, 8:16], bs[:, h, 8:16], cand2[:, h, :], [bcand2, bbs], [bbj])
                        cp("dve", bjf[:], bj[:], [bbj], [bbjf])
                        tt("dve", oh[:], bjf[:].unsqueeze(3).to_broadcast([128, 8, 16, 16]), thrb, ALU.is_ge, [bbjf, bthr], [boh])
                        red("dve", ba[:], oh[:], ALU.add, [boh], [bba])
                        stt("dve", bb_[:], ba[:], -16.0, bjf[:], ALU.mult, ALU.add, [bba, bbjf], [bbb])
                        riv = ri[:].rearrange("p i (h k) -> p i h k", h=8)
                        for half_i, (src_t, bsrc_t) in enumerate(((ba, bba), (bb_, bbb))):
                            tt("dve", oh[:], src_t[:].unsqueeze(3).to_broadcast([128, 8, 16, 16]), io16b, ALU.is_equal, [bsrc_t, bio], [boh])
                            tt("dve", prod[:], oh[:], tifv[:, :, half_i, :].unsqueeze(2).to_broadcast([128, 8, 16, 16]), ALU.mult,
                               [boh, btif], [bprod])
                            red("dve", riv[:, half_i, :, :], prod[:], ALU.add, [bprod], [bri])
                        tt("dve", ex[:], bs[:], bs[:, :, 0:1].to_broadcast([128, 8, 16]), ALU.subtract, [bbs], [bex])
                        act(ex[:], ex[:], AF.Exp, [bex], [bex])
                        red("dve", sm[:], ex[:], ALU.add, [bex], [bsm])
                        vrecip(sm[:], sm[:], [bsm], [bsm])
                        tt("dve", riv[:, 2, :, :], ex[:], sm[:].unsqueeze(2).to_broadcast([128, 8, 16]), ALU.mult, [bex, bsm], [bri])
                        for i in range(3):
                            tr(PS[6][:, i * 128:(i + 1) * 128], ri[:, i, :], identf[:], [bri, bidf], [PSB[6]])
                        act(rtst[gs][:, :, bsl], PS[6][:, 0:384].rearrange("p (i t) -> p i t", i=3), AF.Copy, [PSB[6]], [brt[gs]])
                    P.dma("pool", ghn[gs], hnT_s[:, :, tg0:tg0 + 512].rearrange("c p t -> p c t"), hnst[gs][:], reads=[bhn[gs]])
                    P.dma("pool", grt[gs], rT_s[:, :, tg0:tg0 + 512].rearrange("i p t -> p i t"), rtst[gs][:], reads=[brt[gs]])
                P.barrier()

        if "D" in phases:
            with ExitStack() as sa:
                def sb(name, shape, dt):
                    return sa.enter_context(nc.sbuf_tensor("d_" + name, shape, dt))
                TT = 256
                Gt = sb("Gt", [128, 128, TT], BF16); bGt = Buf()
                hn = [sb("hn%d" % i, [128, 8, TT], BF16) for i in range(2)]; bhn2 = [Buf(), Buf()]; ghn2 = [P.grp("hn2"), P.grp("hn2")]
                rt = [sb("rt%d" % i, [128, 3, TT], F32) for i in range(2)]; brt2 = [Buf(), Buf()]; grt2 = [P.grp("rt2"), P.grp("rt2")]
                hh = [sb("hh%d" % i, [128, 2, 1024], F32) for i in range(2)]; bhh = [Buf(), Buf()]; ghh = [P.grp("hh"), P.grp("hh")]
                gho = [P.grp("ho"), P.grp("ho")]
                Ab = [sb("Ab%d" % i, [128, 16, 128], BF16) for i in range(2)]; bAb = [Buf(), Buf()]
                Bb = [sb("Bb%d" % i, [128, 16, 128], BF16) for i in range(2)]; bBb = [Buf(), Buf()]
                Bp = [sb("Bp%d" % i, [128, 16, 128], BF16) for i in range(2)]; bBp = [Buf(), Buf()]
                NU = 3
                u4 = [sb("u4_%d" % i, [128, 4, 1024], BF16) for i in range(NU)]; bu4 = [Buf() for _ in range(NU)]; gu4 = [P.grp("u4") for _ in range(NU)]
                v4 = [sb("v4_%d" % i, [128, 4, 1024], BF16) for i in range(NU)]; bv4 = [Buf() for _ in range(NU)]; gv4 = [P.grp("v4") for _ in range(NU)]
                NG = 4
                gl = [sb("gl%d" % i, [128, TT], F32) for i in range(NG)]; bgl = [Buf() for _ in range(NG)]
                Wt = [sb("Wt%d" % i, [128, TT], BF16) for i in range(NG)]; bWt = [Buf() for _ in range(NG)]
                io3 = io128[:].unsqueeze(1).to_broadcast([128, 16, 128])
                ucnt = 0
                kcnt = 0
                gcnt = 0
                for tile in range(S // TT):
                    t0 = tile * TT
                    s = tile % 2
                    P.dma("sp", ghn2[s], hn[s][:], hnT_s[:, :, t0:t0 + TT].rearrange("c p t -> p c t"), writes=[bhn2[s]])
                    P.dma("sp", grt2[s], rt[s][:], rT_s[:, :, t0:t0 + TT].rearrange("i p t -> p i t"), writes=[brt2[s]])
                    P.dma("sp", ghh[s], hh[s][:], y[t0:t0 + TT, :].rearrange("(b p) f -> p b f", p=128), writes=[bhh[s]])
                    for sub in range(TT // 16):
                        c0 = sub * 16
                        a = sub % 2
                        tt("dve", Ab[a][:], io3, rt[s][:, 0, c0:c0 + 16].unsqueeze(2).to_broadcast([128, 16, 128]), ALU.is_equal,
                           [bio, brt2[s]], [bAb[a]])
                        tt("dve", Bb[a][:], io3, rt[s][:, 1, c0:c0 + 16].unsqueeze(2).to_broadcast([128, 16, 128]), ALU.is_equal,
                           [bio, brt2[s]], [bBb[a]])
                        tt("pool", Bp[a][:], Bb[a][:], rt[s][:, 2, c0:c0 + 16].unsqueeze(2).to_broadcast([128, 16, 128]), ALU.mult,
                           [bBb[a], brt2[s]], [bBp[a]])
                        for c4 in range(4):
                            bank = 6 + (gcnt % 2)
                            gcnt += 1
                            for c in range(4):
                                cc = c4 * 4 + c
                                mm(PS[bank][:, :].rearrange("p (i c) -> p c i", c=4)[:, c, :], Bp[a][:, cc, :], Ab[a][:, cc, :], True, True,
                                   [bBp[a], bAb[a]], [PSB[bank]])
                            tk = c0 + c4 * 4
                            act(Gt[:, :, tk:tk + 4], PS[bank][:, :].rearrange("p (i c) -> p i c", c=4), AF.Copy,
                                [PSB[bank]], [bGt])
                    def vside(j, k, us, jj):
                        for b in range(2):
                            for hf in range(2):
                                mm(PS[b * 2 + hf][:, :], Wt[k][:, b * 128:(b + 1) * 128], v4[us][:, jj, hf * 512:(hf + 1) * 512],
                                   j == 0, j == 127, [bWt[k], bv4[us]], [PSB[b * 2 + hf]])
                    pend = None
                    for jg in range(32):
                        us = ucnt % NU
                        ucnt += 1
                        P.dma("sp", gu4[us], u4[us][:], uT_s[jg * 4:(jg + 1) * 4].rearrange("j p f -> p j f"), writes=[bu4[us]])
                        P.dma("sp", gv4[us], v4[us][:], v_s[jg * 512:(jg + 1) * 512, :].rearrange("(j n) f -> n j f", n=128), writes=[bv4[us]])
                        for jj in range(4):
                            j = jg * 4 + jj
                            abank = 4 + (j % 4)
                            k = kcnt % NG
                            kcnt += 1
                            for c in range(8):
                                mm(PS[abank][:, 0:TT], u4[us][:, jj, c * 128:(c + 1) * 128], hn[s][:, c, :], c == 0, c == 7,
                                   [bu4[us], bhn2[s]], [PSB[abank]])
                            act(gl[k][:], PS[abank][:, 0:TT], GELU, [PSB[abank]], [bgl[k]])
                            tt("dve", Wt[k][:], gl[k][:], Gt[:, j, :], ALU.mult, [bgl[k], bGt], [bWt[k]])
                            if pend is not None:
                                vside(*pend)
                            pend = (j, k, us, jj)
                    vside(*pend)
                    pend = None
                    for b in range(2):
                        for hf in range(2):
                            tt("dve", hh[s][:, b, hf * 512:(hf + 1) * 512], PS[b * 2 + hf][:, :], hh[s][:, b, hf * 512:(hf + 1) * 512], ALU.add,
                               [PSB[b * 2 + hf], bhh[s]], [bhh[s]])
                    P.dma("pool", gho[s], y[t0:t0 + TT, :].rearrange("(b p) f -> p b f", p=128), hh[s][:], reads=[bhh[s]])
                P.barrier()

        P.emit()
    return nc


def prep_shared(norm1_g, w_in, q_norm_g, k_norm_g, w_pool, pool_scale, w_out, norm2_g, w_query, sub_keys, expert_u, expert_v):
    f = np.float32
    w_in0 = np.asarray(w_in[0], f)
    w_in_r = np.ascontiguousarray(w_in0.reshape(8, 128, 60, 128).transpose(2, 1, 0, 3))
    w_out_r = np.ascontiguousarray(np.asarray(w_out[0], f).reshape(8, 128, 1024).transpose(1, 0, 2))
    w_q_r = np.ascontiguousarray(np.asarray(w_query[0], f).reshape(8, 128, 2048).transpose(1, 0, 2))
    w_pool_r = np.ascontiguousarray(np.asarray(w_pool[0], f).transpose(1, 0, 2))
    skT = np.ascontiguousarray(np.asarray(sub_keys[0], f).reshape(16, 128, 128).transpose(2, 0, 1))
    uT_r = np.ascontiguousarray(np.asarray(expert_u[0], f).reshape(128, 128, 8, 128).transpose(0, 3, 2, 1))
    v_in = np.ascontiguousarray(np.asarray(expert_v[0], f))
    vecs = np.zeros((128, 32), f)
    vecs[:, 0:8] = np.asarray(norm1_g[0], f).reshape(8, 128).T
    vecs[:, 8:16] = np.asarray(norm2_g[0], f).reshape(8, 128).T
    vecs[:, 16:24] = np.asarray(pool_scale[0], f).reshape(8, 128).T
    vecs[:, 24] = np.tile(np.asarray(q_norm_g[0], f), 2)
    vecs[:, 25] = np.tile(np.asarray(k_norm_g[0], f), 2)
    return dict(w_in_r=w_in_r, w_out_r=w_out_r, w_q_r=w_q_r, w_pool_r=w_pool_r, skT=skT, uT_r=uT_r, v_in=v_in, vecs=vecs)


_NC_CACHE = {}


def kernel(x, norm1_g, w_in, q_norm_g, k_norm_g, w_pool, pool_scale, w_out, norm2_g, w_query, sub_keys, expert_u, expert_v):
    x = np.asarray(x, np.float32)
    B, S, D = x.shape
    shared = prep_shared(norm1_g, w_in, q_norm_g, k_norm_g, w_pool, pool_scale, w_out, norm2_g, w_query, sub_keys, expert_u, expert_v)
    if S not in _NC_CACHE:
        _NC_CACHE[S] = build(S)
    nc = _NC_CACHE[S]
    in_maps = []
    for b in range(B):
        m = dict(shared)
        m["x"] = np.ascontiguousarray(x[b])
        in_maps.append(m)
    res = run_bass_kernel_spmd(nc, in_maps, core_ids=list(range(B)))
    return np.stack([np.asarray(r["y"], np.float32) for r in res.results], axis=0)
```

```python
import numpy as np
from contextlib import ExitStack
import concourse.bass as bass
import concourse.mybir as mybir
from concourse.bass_utils import run_bass_kernel_spmd

F32 = mybir.dt.float32
BF16 = mybir.dt.bfloat16
U32 = mybir.dt.uint32
I32 = mybir.dt.int32
AF = mybir.ActivationFunctionType
ALU = mybir.AluOpType
AX = mybir.AxisListType

ENGS = ("pe", "dve", "act", "pool", "sp")
EPOCH = 20000


class Buf:
    __slots__ = ("w", "r", "name")

    def __init__(self, name=""):
        self.w = {}
        self.r = {}
        self.name = name


class Grp:
    __slots__ = ("key", "cnt")

    def __init__(self, key):
        self.key = key
        self.cnt = 0


class Prog:
    def __init__(self, nc, stack):
        self.nc = nc
        self.stack = stack
        self.semh = []
        self.ops = {e: [] for e in ENGS}
        self.cnt = {e: 0 for e in ENGS}
        self.key = {}
        self.ownkeys = {e: set() for e in ENGS}
        self.waited = {e: {} for e in ENGS}
        for e in ENGS:
            self._new_epoch(e)
        self.nops = 0
        self.latest = {}

    def barrier(self):
        for e in ENGS:
            waits = []
            for k, v in self.latest.items():
                if k in self.ownkeys[e]:
                    continue
                if self.waited[e].get(k, 0) >= v:
                    continue
                self.waited[e][k] = v
                waits.append((k, v))
            self.ops[e].append((waits, None, None))

    def _new_sem(self, name):
        h = self.stack.enter_context(self.nc.semaphore(name))
        self.semh.append(h)
        return len(self.semh) - 1

    def _new_epoch(self, e):
        k = self._new_sem("s_%s_%d" % (e, len(self.ownkeys[e])))
        self.key[e] = k
        self.ownkeys[e].add(k)
        self.cnt[e] = 0

    def grp(self, name="g"):
        return Grp(self._new_sem("d_%s_%d" % (name, len(self.semh))))

    def sb(self, name, shape, dt):
        return self.stack.enter_context(self.nc.sbuf_tensor(name, shape, dt))

    def ps(self, name, shape, dt):
        return self.stack.enter_context(self.nc.psum_tensor(name, shape, dt))

    def _deps(self, eng, reads, writes):
        deps = {}
        for b in reads:
            for k, v in b.w.items():
                if deps.get(k, 0) < v:
                    deps[k] = v
        for b in writes:
            for k, v in b.w.items():
                if deps.get(k, 0) < v:
                    deps[k] = v
            for k, v in b.r.items():
                if deps.get(k, 0) < v:
                    deps[k] = v
        waits = []
        wd = self.waited[eng]
        for k, v in deps.items():
            if eng == "pe" and k in self.ownkeys[eng]:
                continue
            if wd.get(k, 0) >= v:
                continue
            wd[k] = v
            waits.append((k, v))
        return waits

    def _mark(self, tok, reads, writes):
        k, v = tok
        self.latest[k] = v
        for b in reads:
            b.r[k] = v
        for b in writes:
            b.w = {k: v}
            b.r = {}

    def op(self, eng, fn, reads=(), writes=()):
        waits = self._deps(eng, reads, writes)
        if self.cnt[eng] >= EPOCH:
            self._new_epoch(eng)
        self.cnt[eng] += 1
        tok = (self.key[eng], self.cnt[eng])
        self.ops[eng].append((waits, fn, (tok[0], 1)))
        self._mark(tok, reads, writes)
        self.nops += 1
        return tok

    def dma(self, eng, grp, out, in_, reads=(), writes=(), **kw):
        waits = self._deps(eng, reads, writes)
        grp.cnt += 16
        tok = (grp.key, grp.cnt)
        self.ops[eng].append((waits, lambda e: e.dma_start(out=out, in_=in_, **kw), (tok[0], 16)))
        self._mark(tok, reads, writes)
        self.nops += 1
        return tok

    def wait_all(self, eng, bufs):
        waits = self._deps(eng, bufs, bufs)
        self.ops[eng].append((waits, None, None))

    def emit(self):
        nc = self.nc
        semh = self.semh
        ops = self.ops

        def replay(name, e):
            for waits, fn, inc in ops[name]:
                for k, v in waits:
                    e.wait_ge(semh[k], v)
                if fn is not None:
                    ins = fn(e)
                    ins.then_inc(semh[inc[0]], inc[1])

        with nc.Block() as block:
            @block.tensor
            def _(e):
                replay("pe", e)

            @block.vector
            def _(e):
                replay("dve", e)

            @block.scalar
            def _(e):
                replay("act", e)

            @block.gpsimd
            def _(e):
                replay("pool", e)

            @block.sync
            def _(e):
                replay("sp", e)


def _mk(P):
    def mm(out, lhsT, rhs, start, stop, reads, writes):
        return P.op("pe", lambda e: e.matmul(out, lhsT=lhsT, rhs=rhs, start=start, stop=stop), reads, writes)
    def tr(out, in_, ident, reads, writes):
        return P.op("pe", lambda e: e.transpose(out=out, in_=in_, identity=ident), reads, writes)
    def act(out, in_, func, reads, writes, **kw):
        return P.op("act", lambda e: e.activation(out=out, in_=in_, func=func, **kw), reads, writes)
    def tt(eng, out, in0, in1, op, reads, writes):
        return P.op(eng, lambda e: e.tensor_tensor(out=out, in0=in0, in1=in1, op=op), reads, writes)
    def ts(eng, out, in0, s1, s2, op0, op1, reads, writes):
        if s2 is None:
            return P.op(eng, lambda e: e.tensor_scalar(out=out, in0=in0, scalar1=s1, scalar2=None, op0=op0), reads, writes)
        return P.op(eng, lambda e: e.tensor_scalar(out=out, in0=in0, scalar1=s1, scalar2=s2, op0=op0, op1=op1), reads, writes)
    def stt(eng, out, in0, scalar, in1, op0, op1, reads, writes):
        return P.op(eng, lambda e: e.scalar_tensor_tensor(out=out, in0=in0, scalar=scalar, in1=in1, op0=op0, op1=op1), reads, writes)
    def cp(eng, out, in_, reads, writes):
        if eng == "act":
            return P.op(eng, lambda e: e.copy(out=out, in_=in_), reads, writes)
        return P.op(eng, lambda e: e.tensor_copy(out=out, in_=in_), reads, writes)
    def ms(eng, ap, val, writes):
        return P.op(eng, lambda e: e.memset(ap, val), (), writes)
    def red(eng, out, in_, op, reads, writes):
        return P.op(eng, lambda e: e.tensor_reduce(out=out, in_=in_, axis=AX.X, op=op), reads, writes)
    P.mm, P.tr, P.act, P.tt, P.ts, P.stt, P.cp, P.ms, P.red = mm, tr, act, tt, ts, stt, cp, ms, red
    return P

import math

EPS = 1e-6
PATS = ((128, 1), (512, 4), (2048, 16))


def alibi_slopes(n):
    def geometric(k):
        start = 2.0 ** (-8.0 / k)
        return [start ** (i + 1) for i in range(k)]
    p = 2 ** int(math.floor(math.log2(n)))
    s = geometric(p) + geometric(2 * p)[0::2][: n - p]
    return np.sort(np.array(s, dtype=np.float32))[::-1].copy()


def build(S=8192, debug=False, phases="ABCD", gelu_func=None):
    nc = bass.Bass("TRN2", target_bir_lowering=False)
    GELU = gelu_func or AF.Gelu_apprx_tanh
    HALF = min(S, 4096)
    NH = S // HALF
    TPH = HALF // 512
    NSB = S // 2048
    skind = "ExternalOutput" if debug else "Internal"

    def din(name, shape, dt=F32):
        return nc.dram_tensor(name, shape, dt, kind="ExternalInput").ap()

    def dscr(name, shape, dt):
        return nc.dram_tensor(name, shape, dt, kind=skind).ap()

    x = din("x", [S, 1024])
    w_in_r = din("w_in_r", [60, 128, 8, 128])
    w_out_r = din("w_out_r", [128, 8, 1024])
    w_q_r = din("w_q_r", [128, 8, 2048])
    w_pool_r = din("w_pool_r", [128, 4, 256])
    skT_in = din("skT", [128, 16, 128])
    uT_r = din("uT_r", [128, 128, 8, 128])
    v_in = din("v_in", [16384, 1024])
    vecs = din("vecs", [128, 32])
    y = nc.dram_tensor("y", [S, 1024], F32, kind="ExternalOutput").ap()

    qT = dscr("qT", [24, 128, S], BF16)
    kT = [dscr("kT%d" % p, [8, 128, S], BF16) for p in range(3)]
    g1T = dscr("g1T", [8, 128, S], BF16)
    pgT = dscr("pgT", [8, 128, S], BF16)
    vaug = dscr("vaug", [S, 2048], BF16)
    mT = dscr("mT", [8, 128, S], BF16)
    hnT_s = dscr("hnT_s", [8, 128, S], BF16)
    rT_s = dscr("rT_s", [3, 128, S], F32)
    uT_s = dscr("uT_s", [128, 128, 1024], BF16)
    v_s = dscr("v_s", [16384, 1024], BF16)

    slopes = alibi_slopes(48).reshape(3, 16)

    with ExitStack() as st:
        P = _mk(Prog(nc, st))
        mm, tr, act, tt, ts, stt, cp, ms, red = P.mm, P.tr, P.act, P.tt, P.ts, P.stt, P.cp, P.ms, P.red

        vec = P.sb("vec", [128, 32], F32); bvec = Buf()
        identf = P.sb("identf", [128, 128], F32); bidf = Buf()
        identb = P.sb("identb", [128, 128], BF16); bidb = Buf()
        bd = P.sb("bd", [128, 128], BF16); bbd = Buf()
        io128 = P.sb("io128", [128, 128], F32); bio = Buf()
        gq = P.sb("gq", [128, 2], F32); bgq = Buf()
        epsc = P.sb("epsc", [128, 1], F32); beps = Buf()
        P.op("pool", lambda e: e.memset(epsc[:], EPS), (), [beps])
        gc = P.grp("c")
        P.dma("sp", gc, vec[:], vecs, writes=[bvec])
        ms("pool", identf[:], 0.0, [bidf])
        P.op("pool", lambda e: e.affine_select(out=identf[:], in_=identf[:], pattern=[[-1, 128]], compare_op=ALU.not_equal,
                                               fill=1.0, base=0, channel_multiplier=1), [bidf], [bidf])
        cp("pool", identb[:], identf[:], [bidf], [bidb])
        ms("pool", bd[:], 0.0, [bbd])
        ms("pool", bd[0:64, 0:64], 1.0 / 64, [bbd])
        ms("pool", bd[64:128, 64:128], 1.0 / 64, [bbd])
        P.op("pool", lambda e: e.iota(io128[:], pattern=[[1, 128]], base=0, channel_multiplier=0,
                                      allow_small_or_imprecise_dtypes=True), (), [bio])
        ts("dve", gq[:, 0:1], vec[:, 24:25], 0.125, None, ALU.mult, None, [bvec], [bgq])
        cp("dve", gq[:, 1:2], vec[:, 25:26], [bvec, bgq], [bgq])
        g1col = vec[:, 0:8]; g2col = vec[:, 8:16]; pscol = vec[:, 16:24]

        PS = [P.ps("ps%d" % i, [128, 512], F32) for i in range(8)]
        PSB = [Buf("ps%d" % i) for i in range(8)]

        def norm_transpose(src_ap, bsrc, ssq, bssq, junk, bjunk, rstd, brstd, xs, bxs, ptr_bank, gcol, dst_ap, bdst):
            ms("pool", ssq, 0.0, [bssq])
            act(junk, src_ap, AF.Square, [bsrc, bssq], [bjunk, bssq], accum_out=ssq)
            act(rstd, ssq, AF.Sqrt, [bssq, beps], [brstd], scale=1.0 / 1024, bias=epsc[:])
            P.op("dve", lambda e: e.reciprocal(out=rstd, in_=rstd), [brstd], [brstd])
            act(xs, src_ap, AF.Copy, [bsrc, brstd], [bxs], scale=rstd)
            pt = PS[ptr_bank].bitcast(BF16)
            for c in range(8):
                tr(pt[:, c * 128:(c + 1) * 128], xs[:, c * 128:(c + 1) * 128], identb[:], [bxs, bidb], [PSB[ptr_bank]])
            tt("dve", dst_ap, pt[:, 0:1024].rearrange("p (c t) -> p c t", c=8),
               gcol.unsqueeze(2).to_broadcast([128, 8, 128]), ALU.mult, [PSB[ptr_bank], bvec], [bdst])

        if "A" in phases:
            with ExitStack() as sa:
                def sb(name, shape, dt):
                    return sa.enter_context(nc.sbuf_tensor("a_" + name, shape, dt))
                xnT = sb("xnT", [128, 8, HALF], BF16); bxn = Buf()
                xts = [sb("xt%d" % i, [128, 1024], F32) for i in range(2)]; bxt = [Buf(), Buf()]; gxt = [P.grp("x"), P.grp("x")]
                junk = sb("junk", [128, 1024], F32); bjunk = Buf()
                ssq = sb("ssq", [128, 1], F32); bssq = Buf()
                rstd1 = sb("rstd1", [128, 1], F32); brstd1 = Buf()
                xs = sb("xs", [128, 1024], BF16); bxs = Buf()
                wst = [sb("wst%d" % i, [128, 8, 128], F32) for i in range(2)]; bwst = [Buf(), Buf()]; gwst = [P.grp("w"), P.grp("w")]
                wbf = [sb("wbf%d" % i, [128, 8, 128], BF16) for i in range(2)]; bwbf = [Buf(), Buf()]
                wv = sb("wv", [128, 8, 1024], BF16); bwv = Buf()
                wpst = sb("wpst", [128, 4, 256], F32); bwpst = Buf()
                wpb = sb("wpb", [128, 4, 256], BF16); bwpb = Buf()
                NST = 4
                stg = [sb("stg%d" % i, [128, 2048], BF16) for i in range(NST)]; bstg = [Buf() for _ in range(NST)]
                gstg = [P.grp("st") for _ in range(NST)]
                kst = [sb("kst%d" % i, [128, 2048], BF16) for i in range(2)]; bkst = [Buf(), Buf()]; gkst = [P.grp("k"), P.grp("k")]
                sq = [sb("sq%d" % i, [128, 512], BF16) for i in range(2)]; bsq = [Buf(), Buf()]
                rs = [sb("rs%d" % i, [128, 512], F32) for i in range(2)]; brs = [Buf(), Buf()]
                abuf = sb("abuf", [128, 528], F32); bab = Buf()
                sA = sb("sA", [128, 528], F32); bsA = Buf()
                sB = sb("sB", [128, 528], F32); bsB = Buf()
                dT = sb("dT", [128, HALF], BF16); bdT = Buf()
                ahalo = sb("ahalo", [128, 4, 16], F32); bah = Buf()
                rc = sb("rc", [128, 4, 16], F32); brc = Buf()
                tmp16 = sb("tmp16", [128, 16], F32); btmp16 = Buf()
                g0s = [sb("g0s%d" % i, [128, 512], BF16) for i in range(2)]; bg0s = [Buf(), Buf()]
                vst = [sb("vst%d" % i, [128, 2048], BF16) for i in range(2)]; bvst = [Buf(), Buf()]; gvst = [P.grp("v"), P.grp("v")]
                cst = [sb("cst%d" % i, [128, 2048], F32) for i in range(2)]; bcst = [Buf(), Buf()]; gcst = [P.grp("cs"), P.grp("cs")]
                cbf = [sb("cbf%d" % i, [128, 2048], BF16) for i in range(2)]; bcbf = [Buf(), Buf()]; gcbf = [P.grp("cb"), P.grp("cb")]

                gA = P.grp("A")
                P.dma("sp", gA, wpst[:], w_pool_r, writes=[bwpst])
                cp("pool", wpb[:], wpst[:], [bwpst], [bwpb])
                ms("pool", ahalo[:], 0.0, [bah])
                for g in range(4):
                    w = PATS and (2, 4, 8, 16)[g]
                    ts("pool", rc[:, g, :], io128[:, 0:16], 1.0, float(w), ALU.add, ALU.min, [bio], [brc])
                P.op("dve", lambda e: e.reciprocal(out=rc[:], in_=rc[:]), [brc], [brc])
                for i in range(2):
                    ms("pool", vst[i][:], 1.0, [bvst[i]])

                p0_items = []
                if "D" in phases:
                    for it in range(64):
                        p0_items.append(("u", it))
                        p0_items.append(("v", it))
                p0_state = {"i": 0}

                def p0_step():
                    i = p0_state["i"]
                    if i >= len(p0_items):
                        return
                    p0_state["i"] = i + 1
                    kind, it = p0_items[i]
                    s = i % 2
                    if kind == "u":
                        src = uT_r[it * 2:(it + 1) * 2].rearrange("j p c n -> p j (c n)")
                        dst = uT_s[it * 2:(it + 1) * 2].rearrange("j p f -> p j f")
                    else:
                        src = v_in[it * 256:(it + 1) * 256, :].rearrange("(j n) f -> n j f", n=128)
                        dst = v_s[it * 256:(it + 1) * 256, :].rearrange("(j n) f -> n j f", n=128)
                    P.dma("sp", gcst[s], cst[s][:].rearrange("p (j f) -> p j f", j=2), src, writes=[bcst[s]])
                    cp("pool" if (i % 4) < 2 else "act", cbf[s][:], cst[s][:], [bcst[s]], [bcbf[s]])
                    P.dma("pool", gcbf[s], dst, cbf[s][:].rearrange("p (j f) -> p j f", j=2), reads=[bcbf[s]])

                mmcnt = [0]
                stgcnt = [0]
                pendA = [None]
                for half in range(NH):
                    h0 = half * HALF
                    for b in range(HALF // 128):
                        s = b % 2
                        t0 = h0 + b * 128
                        P.dma("sp", gxt[s], xts[s][:], x[t0:t0 + 128, :], writes=[bxt[s]])
                        norm_transpose(xts[s][:], bxt[s], ssq[:], bssq, junk[:], bjunk, rstd1[:], brstd1, xs[:], bxs, 7, g1col,
                                       xnT[:, :, b * 128:(b + 1) * 128], bxn)
                    for k in range(8):
                        s = k % 2
                        P.dma("sp", gwst[s], wst[s][:], w_in_r[36 + k], writes=[bwst[s]])
                        cp("pool", wv[:, :, k * 128:(k + 1) * 128], wst[s][:], [bwst[s]], [bwv])
                    for b in range(HALF // 128):
                        s = b % 2
                        t0 = h0 + b * 128
                        vv = vst[s][:].rearrange("p (q j e) -> p q j e", q=8, j=2)
                        for hf in range(2):
                            bank = 5 + hf
                            for c in range(8):
                                mm(PS[bank][:, :], xnT[:, c, b * 128:(b + 1) * 128], wv[:, c, hf * 512:(hf + 1) * 512],
                                   c == 0, c == 7, [bxn, bwv], [PSB[bank]])
                            pv = PS[bank][:, :].rearrange("p (q j e) -> p q j e", q=4, j=2)
                            act(vv[:, hf * 4:(hf + 1) * 4, 0, 0:64], pv[:, :, 0, :], AF.Copy, [PSB[bank]], [bvst[s]])
                            act(vv[:, hf * 4:(hf + 1) * 4, 1, 64:128], pv[:, :, 1, :], AF.Copy, [PSB[bank]], [bvst[s]])
                        P.dma("pool", gvst[s], vaug[t0:t0 + 128, :], vst[s][:], reads=[bvst[s]])

                    order = []
                    for g in range(4):
                        order.append(("a", g, g))
                        for oc in range(2):
                            order.append(("y", 44 + g * 2 + oc, g * 2 + oc))
                    for qc in range(24):
                        order.append(("q", 4 + qc, qc))
                    for kc in range(8):
                        order.append(("k", 28 + kc, kc))
                    for c in range(8):
                        order.append(("g1", 52 + c, c))
                    def load_w(oi_):
                        ws_ = oi_ % 2
                        P.dma("sp", gwst[ws_], wst[ws_][:], w_in_r[order[oi_][1]], writes=[bwst[ws_]])
                        cp("pool", wbf[ws_][:], wst[ws_][:], [bwst[ws_]], [bwbf[ws_]])
                    load_w(0)
                    for oi, (typ, fc, idx) in enumerate(order):
                        ws = oi % 2
                        if oi + 1 < len(order):
                            load_w(oi + 1)
                        p0_step()
                        for tti in range(TPH):
                            bank = (0, 1, 4, 5, 6)[mmcnt[0] % 5]
                            mmcnt[0] += 1
                            tsl = slice(tti * 512, (tti + 1) * 512)
                            for c in range(8):
                                mm(PS[bank][:, :], wbf[ws][:, c, :], xnT[:, c, tsl], c == 0, c == 7, [bwbf[ws], bxn], [PSB[bank]])
                            def post_tile(typ=typ, idx=idx, tti=tti, bank=bank, half=half, h0=h0, tsl=tsl):
                                pp = PS[bank][:, :]
                                tl = tti % 4
                                sbi = (h0 // 2048) + tti // 4
                                if typ == "a":
                                    g = idx
                                    wsz = (2, 4, 8, 16)[g]
                                    if tti == 0:
                                        cp("pool", abuf[:, 0:16], ahalo[:, g, :], [bah], [bab])
                                    act(abuf[:, 16:528], pp, AF.Copy, [PSB[bank]], [bab])
                                    cur, bcur = abuf, bab
                                    srcs = [(sA, bsA), (sB, bsB)]
                                    off = 0
                                    for stp in range(g + 1):
                                        sh = 1 << stp
                                        off += sh
                                        dst, bdst = srcs[stp % 2]
                                        tt("pool", dst[:, off:528], cur[:, off:528], cur[:, off - sh:528 - sh], ALU.add, [bcur], [bdst])
                                        cur, bcur = dst, bdst
                                    stt("dve", dT[:, tsl], cur[:, 16:528], 1.0 / wsz, abuf[:, 16:528], ALU.mult, ALU.subtract,
                                        [bcur, bab], [bdT])
                                    if half == 0 and tti == 0:
                                        tt("pool", tmp16[:], cur[:, 16:32], rc[:, g, :], ALU.mult, [bcur, brc], [btmp16])
                                        tt("pool", dT[:, 0:16], tmp16[:], abuf[:, 16:32], ALU.subtract, [btmp16, bab], [bdT])
                                    if tti == TPH - 1:
                                        cp("pool", ahalo[:, g, :], abuf[:, 512:528], [bab], [bah])
                                    else:
                                        cp("pool", abuf[:, 0:16], abuf[:, 512:528], [bab], [bab])
                                elif typ == "y":
                                    yc = idx
                                    g, oc = yc // 2, yc % 2
                                    gs = tti % 2
                                    act(g0s[gs][:], pp, AF.Sigmoid, [PSB[bank]], [bg0s[gs]])
                                    ybank = 2 + (tti % 2)
                                    mm(PS[ybank][:, :], wpb[:, g, oc * 128:(oc + 1) * 128], dT[:, tsl], True, True, [bwpb, bdT], [PSB[ybank]])
                                    si = stgcnt[0] % NST
                                    stt("dve", stg[si][:, tl * 512:(tl + 1) * 512], PS[ybank][:, :], pscol[:, yc:yc + 1], g0s[gs][:],
                                        ALU.mult, ALU.mult, [PSB[ybank], bg0s[gs], bvec], [bstg[si]])
                                    if tl == 3:
                                        P.dma("pool", gstg[si], pgT[yc, :, sbi * 2048:(sbi + 1) * 2048], stg[si][:], reads=[bstg[si]])
                                        stgcnt[0] += 1
                                elif typ in ("q", "k"):
                                    ss_ = tti % 2
                                    act(sq[ss_][:], pp, AF.Square, [PSB[bank]], [bsq[ss_]])
                                    sbank = 2 + (tti % 2)
                                    mm(PS[sbank][:, :], bd[:], sq[ss_][:], True, True, [bbd, bsq[ss_]], [PSB[sbank]])
                                    act(rs[ss_][:], PS[sbank][:, :], AF.Sqrt, [PSB[sbank], beps], [brs[ss_]], bias=epsc[:])
                                    P.op("dve", (lambda t: (lambda e: e.reciprocal(out=t, in_=t)))(rs[ss_][:]), [brs[ss_]], [brs[ss_]])
                                    gcol = gq[:, 0:1] if typ == "q" else gq[:, 1:2]
                                    if typ == "q":
                                        p = idx // 8
                                        d = PATS[p][1]
                                        si = stgcnt[0] % NST
                                        if d == 1:
                                            o_ap = stg[si][:, tl * 512:(tl + 1) * 512]
                                            i0_ap, i1_ap = pp, rs[ss_][:]
                                        elif d == 4:
                                            o_ap = stg[si][:, tl * 512:(tl + 1) * 512].rearrange("p (r i) -> p i r", r=4)
                                            i0_ap = pp.rearrange("p (i r) -> p i r", r=4)
                                            i1_ap = rs[ss_][:].rearrange("p (i r) -> p i r", r=4)
                                        else:
                                            o_ap = stg[si][:, :].rearrange("p (r i) -> p i r", r=16)[:, tl * 32:(tl + 1) * 32, :]
                                            i0_ap = pp.rearrange("p (i r) -> p i r", r=16)
                                            i1_ap = rs[ss_][:].rearrange("p (i r) -> p i r", r=16)
                                        stt("dve", o_ap, i0_ap, gcol, i1_ap, ALU.mult, ALU.mult, [PSB[bank], brs[ss_], bgq], [bstg[si]])
                                        if tl == 3:
                                            P.dma("pool", gstg[si], qT[idx, :, sbi * 2048:(sbi + 1) * 2048], stg[si][:], reads=[bstg[si]])
                                            stgcnt[0] += 1
                                    else:
                                        si = stgcnt[0] % NST
                                        nat = stg[si][:, tl * 512:(tl + 1) * 512]
                                        stt("dve", nat, pp, gcol, rs[ss_][:], ALU.mult, ALU.mult, [PSB[bank], brs[ss_], bgq], [bstg[si]])
                                        cp("pool", kst[0][:, tl * 512:(tl + 1) * 512].rearrange("p (r i) -> p i r", r=4),
                                           nat.rearrange("p (i r) -> p i r", r=4), [bstg[si]], [bkst[0]])
                                        cp("pool", kst[1][:, :].rearrange("p (r i) -> p i r", r=16)[:, tl * 32:(tl + 1) * 32, :],
                                           nat.rearrange("p (i r) -> p i r", r=16), [bstg[si]], [bkst[1]])
                                        if tl == 3:
                                            ssl = slice(sbi * 2048, (sbi + 1) * 2048)
                                            P.dma("pool", gstg[si], kT[0][idx, :, ssl], stg[si][:], reads=[bstg[si]])
                                            P.dma("pool", gkst[0], kT[1][idx, :, ssl], kst[0][:], reads=[bkst[0]])
                                            P.dma("pool", gkst[1], kT[2][idx, :, ssl], kst[1][:], reads=[bkst[1]])
                                            stgcnt[0] += 1
                                else:
                                    si = stgcnt[0] % NST
                                    act(stg[si][:, tl * 512:(tl + 1) * 512], pp, AF.Sigmoid, [PSB[bank]], [bstg[si]])
                                    if tl == 3:
                                        P.dma("pool", gstg[si], g1T[idx, :, sbi * 2048:(sbi + 1) * 2048], stg[si][:], reads=[bstg[si]])
                                        stgcnt[0] += 1
                            if pendA[0] is not None:
                                pendA[0]()
                            pendA[0] = post_tile
                    if pendA[0] is not None:
                        pendA[0]()
                        pendA[0] = None
                while p0_state["i"] < len(p0_items):
                    p0_step()
                P.barrier()

        if "B" in phases:
            with ExitStack() as sa:
                def sb(name, shape, dt):
                    return sa.enter_context(nc.sbuf_tensor("b_" + name, shape, dt))
                M = sb("M", [128, 48, 256], BF16); bM = Buf()
                tq = sb("tq", [128, 128], F32); btq = Buf()
                tA = sb("tA", [128, 128], F32); btA = Buf()
                tB = sb("tB", [128, 128], F32); btB = Buf()
                tC = sb("tC", [128, 128], F32); btC = Buf()
                P.op("pool", lambda e: e.iota(tq[:], pattern=[[1, 128]], base=0, channel_multiplier=-1,
                                              allow_small_or_imprecise_dtypes=True), (), [btq])
                for p in range(3):
                    d = PATS[p][1]
                    for h in range(16):
                        c = float(slopes[p, h]) * d
                        ph = p * 16 + h
                        ts("pool", tA[:], tq[:], 0.0, None, ALU.max, None, [btq], [btA])
                        act(tB[:], tA[:], AF.Exp, [btA], [btB], scale=-c)
                        P.op("pool", lambda e: e.affine_select(out=tC[:], in_=tB[:], pattern=[[1, 128]], compare_op=ALU.is_ge,
                                                               fill=0.0, base=0, channel_multiplier=-1), [btB], [btC])
                        cp("dve", M[:, ph, 128:256], tC[:], [btC], [bM])
                        ts("pool", tA[:], tq[:], 0.0, 128.0, ALU.min, ALU.add, [btq], [btA])
                        act(tB[:], tA[:], AF.Exp, [btA], [btB], scale=-c)
                        P.op("pool", lambda e: e.affine_select(out=tC[:], in_=tB[:], pattern=[[-1, 128]], compare_op=ALU.is_ge,
                                                               fill=0.0, base=0, channel_multiplier=1), [btB], [btC])
                        cp("dve", M[:, ph, 0:128], tC[:], [btC], [bM])
                q3 = sb("q3", [128, 3, 2048], BF16); bq3 = Buf(); gq3 = P.grp("q3")
                k3 = [sb("k3_%d" % i, [128, 3, 2048], BF16) for i in range(2)]; bk3 = [Buf(), Buf()]; gk3 = [P.grp("k3"), P.grp("k3")]
                va = [sb("va_%d" % i, [128, 3, 16, 256], BF16) for i in range(2)]; bva = [Buf(), Buf()]; gva = [P.grp("va"), P.grp("va")]
                g1c = sb("g1c", [128, 2048], BF16); bg1c = Buf(); gg1 = P.grp("g1c")
                pgc = sb("pgc", [128, 2048], BF16); bpgc = Buf(); gpg = P.grp("pgc")
                Oacc = [sb("Oacc%d" % i, [128, 2048], F32) for i in range(2)]; bOa = [Buf(), Buf()]
                att = sb("att", [128, 2048], F32); batt = Buf()
                rd = sb("rd", [128, 2048], F32); brd = Buf()
                mc = sb("mc", [128, 2048], BF16); bmc = Buf(); gmc = P.grp("mc")
                NE = 4
                E = [sb("E%d" % i, [128, 512], F32) for i in range(NE)]; bE = [Buf() for _ in range(NE)]
                PT = [sb("PT%d" % i, [128, 512], BF16) for i in range(NE)]; bPT = [Buf() for _ in range(NE)]
                ecnt = [0]
                ocnt = [0]

                def vrecip_b(out, in_, reads, writes):
                    return P.op("dve", lambda e: e.reciprocal(out=out, in_=in_), reads, writes)
                for hp in range(8):
                    for sbi in range(NSB):
                        cur = sbi % 2
                        prv = 1 - cur
                        ssl = slice(sbi * 2048, (sbi + 1) * 2048)
                        for p in range(3):
                            P.dma("sp", gq3, q3[:, p, :], qT[p * 8 + hp, :, ssl], writes=[bq3])
                            P.dma("sp", gk3[cur], k3[cur][:, p, :], kT[p][hp, :, ssl], writes=[bk3[cur]])
                            d = PATS[p][1]
                            nb = 16 // d
                            if d == 1:
                                src = vaug[sbi * 2048:(sbi + 1) * 2048, hp * 256:(hp + 1) * 256].rearrange("(n i) f -> i n f", i=128)
                                P.dma("sp", gva[cur], va[cur][:, p, :, :], src, writes=[bva[cur]])
                            else:
                                for n in range(nb):
                                    r0 = sbi * 2048 + n * 128 * d
                                    src = vaug[r0:r0 + 128 * d, hp * 256:(hp + 1) * 256].rearrange("(i r) f -> i r f", r=d)
                                    P.dma("sp", gva[cur], va[cur][:, p, n * d:(n + 1) * d, :], src, writes=[bva[cur]])
                        P.dma("sp", gg1, g1c[:], g1T[hp, :, ssl], writes=[bg1c])
                        P.dma("sp", gpg, pgc[:], pgT[hp, :, ssl], writes=[bpgc])
                        def make_item(hh, p, grp4, cp2, obank, sbank, ei, cur, prv, sbi, hp):
                            h = hp * 2 + hh
                            rows = slice(hh * 64, hh * 64 + 64)
                            d = PATS[p][1]
                            ph = p * 16 + h
                            pS = PS[sbank][:, :].rearrange("p (c j q) -> p c j q", c=2, j=2)
                            Ev = E[ei][:].rearrange("p (c j q) -> p c j q", c=2, j=2)
                            PTv = PT[ei][:].rearrange("p (c j q) -> p c j q", c=2, j=2)
                            infos = []
                            for j in range(2):
                                cl = grp4 * 4 + cp2 * 2 + j
                                if cl >= d:
                                    infos.append((cl, k3[cur][rows, p, (cl - d) * 128:(cl - d + 1) * 128],
                                                  va[cur][:, p, cl - d, hh * 128:(hh + 1) * 128], bk3[cur], bva[cur]))
                                elif sbi > 0:
                                    infos.append((cl, k3[prv][rows, p, (cl + 16 - d) * 128:(cl + 17 - d) * 128],
                                                  va[prv][:, p, cl + 16 - d, hh * 128:(hh + 1) * 128], bk3[prv], bva[prv]))
                                else:
                                    infos.append((cl, None, None, None, None))

                            def qk():
                                for j in range(2):
                                    cl, kp_ap, vp_ap, bkp, bvp = infos[j]
                                    qap = q3[rows, p, cl * 128:(cl + 1) * 128]
                                    kc_ap = k3[cur][rows, p, cl * 128:(cl + 1) * 128]
                                    mm(pS[:, j, 1, :], kc_ap, qap, True, True, [bk3[cur], bq3], [PSB[sbank]])
                                    if kp_ap is not None:
                                        mm(pS[:, j, 0, :], kp_ap, qap, True, True, [bkp, bq3], [PSB[sbank]])

                            def post():
                                allprev = all(i[1] is not None for i in infos)
                                if allprev:
                                    act(E[ei][:], PS[sbank][:, :], AF.Exp, [PSB[sbank]], [bE[ei]])
                                    tt("dve", PTv, Ev, M[:, ph, :].rearrange("p (j q) -> p j q", j=2).unsqueeze(1).to_broadcast([128, 2, 2, 128]),
                                       ALU.mult, [bE[ei], bM], [bPT[ei]])
                                else:
                                    act(Ev[:, :, 1, :], pS[:, :, 1, :], AF.Exp, [PSB[sbank]], [bE[ei]])
                                    tt("dve", PTv[:, :, 1, :], Ev[:, :, 1, :], M[:, ph, 128:256].unsqueeze(1).to_broadcast([128, 2, 128]),
                                       ALU.mult, [bE[ei], bM], [bPT[ei]])
                                    for j in range(2):
                                        if infos[j][1] is not None:
                                            act(Ev[:, j, 0, :], pS[:, j, 0, :], AF.Exp, [PSB[sbank]], [bE[ei]])
                                            tt("dve", PTv[:, j, 0, :], Ev[:, j, 0, :], M[:, ph, 0:128], ALU.mult, [bE[ei], bM], [bPT[ei]])

                            def pv():
                                pO = PS[obank][:, :].rearrange("p (c q) -> p c q", c=4)
                                for j in range(2):
                                    cl, kp_ap, vp_ap, bkp, bvp = infos[j]
                                    oj = cp2 * 2 + j
                                    vc_ap = va[cur][:, p, cl, hh * 128:(hh + 1) * 128]
                                    if vp_ap is not None:
                                        mm(pO[:, oj, :], vp_ap, PTv[:, j, 0, :], True, False, [bvp, bPT[ei]], [PSB[obank]])
                                        mm(pO[:, oj, :], vc_ap, PTv[:, j, 1, :], False, True, [bva[cur], bPT[ei]], [PSB[obank]])
                                    else:
                                        mm(pO[:, oj, :], vc_ap, PTv[:, j, 1, :], True, True, [bva[cur], bPT[ei]], [PSB[obank]])
                                if cp2 == 1:
                                    if d == 1:
                                        oap = Oacc[hh][:, grp4 * 512:(grp4 + 1) * 512].rearrange("p (c q) -> p c q", c=4)
                                    elif d == 4:
                                        oap = Oacc[hh][:, grp4 * 512:(grp4 + 1) * 512].rearrange("p (i r) -> p r i", r=4)
                                    else:
                                        oap = Oacc[hh][:, :].rearrange("p (i r) -> p r i", r=16)[:, grp4 * 4:(grp4 + 1) * 4, :]
                                    if p == 0:
                                        cp("dve", oap, pO, [PSB[obank]], [bOa[hh]])
                                    else:
                                        tt("dve", oap, oap, pO, ALU.add, [PSB[obank], bOa[hh]], [bOa[hh]])
                                if p == 2 and grp4 == 3 and cp2 == 1:
                                    if hh == 0:
                                        num, den = Oacc[0][0:64, :], Oacc[0][64:128, :]
                                    else:
                                        num, den = Oacc[1][64:128, :], Oacc[1][0:64, :]
                                    vrecip_b(rd[rows, :], den, [bOa[hh]], [brd])
                                    tt("dve", att[rows, :], num, rd[rows, :], ALU.mult, [bOa[hh], brd], [batt])
                                    tt("pool", att[rows, :], att[rows, :], g1c[rows, :], ALU.mult, [batt, bg1c], [batt])
                                    tt("pool", mc[rows, :], att[rows, :], pgc[rows, :], ALU.add, [batt, bpgc], [bmc])
                            return qk, post, pv

                        items = []
                        for hh in range(2):
                            for p in range(3):
                                for grp4 in range(4):
                                    obank = (3, 4, 6)[ocnt[0] % 3]
                                    ocnt[0] += 1
                                    for cp2 in range(2):
                                        sbank = (0, 1, 2, 5)[ecnt[0] % 4]
                                        ei = ecnt[0] % NE
                                        ecnt[0] += 1
                                        items.append(make_item(hh, p, grp4, cp2, obank, sbank, ei, cur, prv, sbi, hp))
                        for ii, (qk, post, pv) in enumerate(items):
                            qk()
                            post()
                            if ii > 0:
                                items[ii - 1][2]()
                        items[-1][2]()
                        P.dma("pool", gmc, mT[hp, :, ssl], mc[:], reads=[bmc])
                P.barrier()

        def vmax(out, in_, reads, writes):
            return P.op("dve", lambda e: e.max(out=out, in_=in_), reads, writes)

        def vmaxidx(out, in_max, in_values, reads, writes):
            return P.op("dve", lambda e: e.max_index(out=out, in_max=in_max, in_values=in_values), reads, writes)

        def vmr(out, rep, vals, reads, writes):
            return P.op("dve", lambda e: e.match_replace(out=out, in_to_replace=rep, in_values=vals, imm_value=-1e30), reads, writes)

        def vrecip(out, in_, reads, writes):
            return P.op("dve", lambda e: e.reciprocal(out=out, in_=in_), reads, writes)

        if "C" in phases:
            with ExitStack() as sa:
                def sb(name, shape, dt):
                    return sa.enter_context(nc.sbuf_tensor("c_" + name, shape, dt))
                woutb = sb("woutb", [128, 8, 1024], BF16); bwo = Buf()
                wqb = sb("wqb", [128, 8, 2048], BF16); bwq = Buf()
                skb = sb("skb", [128, 16, 128], BF16); bsk = Buf()
                wl = [sb("wl%d" % i, [128, 2048], F32) for i in range(2)]; bwl = [Buf(), Buf()]; gwl = [P.grp("wl"), P.grp("wl")]
                pieces = []
                for i in range(4):
                    pieces.append((w_out_r[:, 2 * i:2 * i + 2, :].rearrange("p c f -> p (c f)"),
                                   woutb[:, 2 * i:2 * i + 2, :].rearrange("p c f -> p (c f)"), bwo))
                for i in range(8):
                    pieces.append((w_q_r[:, i, :], wqb[:, i, :], bwq))
                pieces.append((skT_in.rearrange("p h n -> p (h n)"), skb[:].rearrange("p h n -> p (h n)"), bsk))
                for i, (src, dst, bd_) in enumerate(pieces):
                    s = i % 2
                    P.dma("sp", gwl[s], wl[s][:], src, writes=[bwl[s]])
                    cp("pool" if i % 2 else "act", dst, wl[s][:], [bwl[s]], [bd_])
                mt = [sb("mt%d" % i, [128, 8, 512], BF16) for i in range(2)]; bmt = [Buf(), Buf()]; gmt = [P.grp("mt"), P.grp("mt")]
                xt2 = [sb("xt2_%d" % i, [128, 1024], F32) for i in range(2)]; bxt2 = [Buf(), Buf()]; gxt2 = [P.grp("x2"), P.grp("x2")]
                ht = [sb("ht%d" % i, [128, 1024], F32) for i in range(2)]; bht = [Buf(), Buf()]; ght = [P.grp("ht"), P.grp("ht")]
                junk = sb("junk", [128, 1024], F32); bjunk = Buf()
                ssq = sb("ssq", [128, 1], F32); bssq = Buf()
                rstd1 = sb("rstd1", [128, 1], F32); brstd1 = Buf()
                xs = sb("xs", [128, 1024], BF16); bxs = Buf()
                hnst = [sb("hnst%d" % i, [128, 8, 512], BF16) for i in range(2)]; bhn = [Buf(), Buf()]; ghn = [P.grp("hn"), P.grp("hn")]
                rtst = [sb("rtst%d" % i, [128, 3, 512], F32) for i in range(2)]; brt = [Buf(), Buf()]; grt = [P.grp("rt"), P.grp("rt")]
                qpT = sb("qpT", [128, 16, 128], BF16); bqp = Buf()
                sc = sb("sc", [128, 16, 128], F32); bsc = Buf()
                sc2 = sb("sc2", [128, 16, 128], F32); bsc2 = Buf()
                ts1 = sb("ts1", [128, 16, 16], F32); bts1 = Buf()
                ti1 = sb("ti1", [128, 16, 16], U32); bti1 = Buf()
                tif = sb("tif", [128, 16, 16], F32); btif = Buf()
                cand = sb("cand", [128, 8, 256], F32); bcand = Buf()
                cand2 = sb("cand2", [128, 8, 256], F32); bcand2 = Buf()
                bs = sb("bs", [128, 8, 16], F32); bbs = Buf()
                bj = sb("bj", [128, 8, 16], U32); bbj = Buf()
                bjf = sb("bjf", [128, 8, 16], F32); bbjf = Buf()
                ba = sb("ba", [128, 8, 16], F32); bba = Buf()
                bb_ = sb("bb", [128, 8, 16], F32); bbb = Buf()
                oh = sb("oh", [128, 8, 16, 16], F32); boh = Buf()
                prod = sb("prod", [128, 8, 16, 16], F32); bprod = Buf()
                ri = sb("ri", [128, 3, 128], F32); bri = Buf()
                ex = sb("ex", [128, 8, 16], F32); bex = Buf()
                sm = sb("sm", [128, 8], F32); bsm = Buf()
                io16b = io128[:, 0:16].unsqueeze(1).unsqueeze(1).to_broadcast([128, 8, 16, 16])
                thr = sb("thr", [128, 16], F32); bthr = Buf()
                P.op("pool", lambda e: e.iota(thr[:], pattern=[[16, 16]], base=16, channel_multiplier=0,
                                              allow_small_or_imprecise_dtypes=True), (), [bthr])
                thrb = thr[:].unsqueeze(1).unsqueeze(1).to_broadcast([128, 8, 16, 16])
                for grp_i in range(S // 512):
                    gs = grp_i % 2
                    tg0 = grp_i * 512
                    P.dma("sp", gmt[gs], mt[gs][:], mT[:, :, tg0:tg0 + 512].rearrange("c p t -> p c t"), writes=[bmt[gs]])
                    for b in range(4):
                        blk = grp_i * 4 + b
                        s = blk % 2
                        t0 = tg0 + b * 128
                        bsl = slice(b * 128, (b + 1) * 128)
                        P.dma("sp", gxt2[s], xt2[s][:], x[t0:t0 + 128, :], writes=[bxt2[s]])
                        for hf in range(2):
                            for c in range(8):
                                mm(PS[hf][:, :], mt[gs][:, c, bsl], woutb[:, c, hf * 512:(hf + 1) * 512], c == 0, c == 7,
                                   [bmt[gs], bwo], [PSB[hf]])
                        for hf in range(2):
                            tt("dve", ht[s][:, hf * 512:(hf + 1) * 512], PS[hf][:, :], xt2[s][:, hf * 512:(hf + 1) * 512], ALU.add,
                               [PSB[hf], bxt2[s]], [bht[s]])
                        P.dma("pool", ght[s], y[t0:t0 + 128, :], ht[s][:], reads=[bht[s]])
                        norm_transpose(ht[s][:], bht[s], ssq[:], bssq, junk[:], bjunk, rstd1[:], brstd1, xs[:], bxs, 7, g2col,
                                       hnst[gs][:, :, bsl], bhn[gs])
                        for j in range(16):
                            bank = 2 + j // 4
                            for c in range(8):
                                mm(PS[bank][:, (j % 4) * 128:(j % 4 + 1) * 128], wqb[:, c, j * 128:(j + 1) * 128], hnst[gs][:, c, bsl],
                                   c == 0, c == 7, [bwq, bhn[gs]], [PSB[bank]])
                        for g4 in range(4):
                            act(qpT[:, g4 * 4:(g4 + 1) * 4, :].rearrange("p j t -> p (j t)"), PS[2 + g4][:, :], AF.Copy, [PSB[2 + g4]], [bqp])
                        for j in range(16):
                            bank = 2 + j // 4
                            mm(PS[bank][:, (j % 4) * 128:(j % 4 + 1) * 128], qpT[:, j, :], skb[:, j, :], True, True, [bqp, bsk], [PSB[bank]])
                        for g4 in range(4):
                            act(sc[:, g4 * 4:(g4 + 1) * 4, :].rearrange("p j t -> p (j t)"), PS[2 + g4][:, :], AF.Copy, [PSB[2 + g4]], [bsc])
                        for j in range(16):
                            vmax(ts1[:, j, 0:8], sc[:, j, :], [bsc], [bts1])
                            vmaxidx(ti1[:, j, 0:8], ts1[:, j, 0:8], sc[:, j, :], [bsc, bts1], [bti1])
                            vmr(sc2[:, j, :], ts1[:, j, 0:8], sc[:, j, :], [bsc, bts1], [bsc2])
                            vmax(ts1[:, j, 8:16], sc2[:, j, :], [bsc2], [bts1])
                            vmaxidx(ti1[:, j, 8:16], ts1[:, j, 8:16], sc2[:, j, :], [bsc2, bts1], [bti1])
                        cp("dve", tif[:], ti1[:], [bti1], [btif])
                        ts1v = ts1[:].rearrange("p (h t) k -> p h t k", t=2)
                        tifv = tif[:].rearrange("p (h t) k -> p h t k", t=2)
                        tt("dve", cand[:].rearrange("p h (a b) -> p h a b", a=16),
                           ts1v[:, :, 0, :].unsqueeze(3).to_broadcast([128, 8, 16, 16]),
                           ts1v[:, :, 1, :].unsqueeze(2).to_broadcast([128, 8, 16, 16]), ALU.add, [bts1], [bcand])
                        for h in range(8):
                            vmax(bs[:, h, 0:8], cand[:, h, :], [bcand], [bbs])
                            vmaxidx(bj[:, h, 0:8], bs[:, h, 0:8], cand[:, h, :], [bcand, bbs], [bbj])
                            vmr(cand2[:, h, :], bs[:, h, 0:8], cand[:, h, :], [bcand, bbs], [bcand2])
                            vmax(bs[:, h, 8:16], cand2[:, h, :], [bcand2], [bbs])
                            vmaxidx(bj[:, h, 8:16], bs[:, h, 8:16], cand2[:, h, :], [bcand2, bbs], [bbj])
                        cp("dve", bjf[:], bj[:], [bbj], [bbjf])
                        tt("dve", oh[:], bjf[:].unsqueeze(3).to_broadcast([128, 8, 16, 16]), thrb, ALU.is_ge, [bbjf, bthr], [boh])
                        red("dve", ba[:], oh[:], ALU.add, [boh], [bba])
                        stt("dve", bb_[:], ba[:], -16.0, bjf[:], ALU.mult, ALU.add, [bba, bbjf], [bbb])
                        riv = ri[:].rearrange("p i (h k) -> p i h k", h=8)
                        for half_i, (src_t, bsrc_t) in enumerate(((ba, bba), (bb_, bbb))):
                            tt("dve", oh[:], src_t[:].unsqueeze(3).to_broadcast([128, 8, 16, 16]), io16b, ALU.is_equal, [bsrc_t, bio], [boh])
                            tt("pool", prod[:], oh[:], tifv[:, :, half_i, :].unsqueeze(2).to_broadcast([128, 8, 16, 16]), ALU.mult,
                               [boh, btif], [bprod])
                            red("dve", riv[:, half_i, :, :], prod[:], ALU.add, [bprod], [bri])
                        tt("dve", ex[:], bs[:], bs[:, :, 0:1].to_broadcast([128, 8, 16]), ALU.subtract, [bbs], [bex])
                        act(ex[:], ex[:], AF.Exp, [bex], [bex])
                        red("dve", sm[:], ex[:], ALU.add, [bex], [bsm])
                        vrecip(sm[:], sm[:], [bsm], [bsm])
                        tt("dve", riv[:, 2, :, :], ex[:], sm[:].unsqueeze(2).to_broadcast([128, 8, 16]), ALU.mult, [bex, bsm], [bri])
                        for i in range(3):
                            tr(PS[6][:, i * 128:(i + 1) * 128], ri[:, i, :], identf[:], [bri, bidf], [PSB[6]])
                        act(rtst[gs][:, :, bsl], PS[6][:, 0:384].rearrange("p (i t) -> p i t", i=3), AF.Copy, [PSB[6]], [brt[gs]])
                    P.dma("pool", ghn[gs], hnT_s[:, :, tg0:tg0 + 512].rearrange("c p t -> p c t"), hnst[gs][:], reads=[bhn[gs]])
                    P.dma("pool", grt[gs], rT_s[:, :, tg0:tg0 + 512].rearrange("i p t -> p i t"), rtst[gs][:], reads=[brt[gs]])
                P.barrier()

        if "D" in phases:
            with ExitStack() as sa:
                def sb(name, shape, dt):
                    return sa.enter_context(nc.sbuf_tensor("d_" + name, shape, dt))
                TT = 256
                Gt = sb("Gt", [128, 128, TT], BF16); bGt = Buf()
                hn = [sb("hn%d" % i, [128, 8, TT], BF16) for i in range(2)]; bhn2 = [Buf(), Buf()]; ghn2 = [P.grp("hn2"), P.grp("hn2")]
                rt = [sb("rt%d" % i, [128, 3, TT], F32) for i in range(2)]; brt2 = [Buf(), Buf()]; grt2 = [P.grp("rt2"), P.grp("rt2")]
                hh = [sb("hh%d" % i, [128, 2, 1024], F32) for i in range(2)]; bhh = [Buf(), Buf()]; ghh = [P.grp("hh"), P.grp("hh")]
                gho = [P.grp("ho"), P.grp("ho")]
                Ab = [sb("Ab%d" % i, [128, 16, 128], BF16) for i in range(2)]; bAb = [Buf(), Buf()]
                Bb = [sb("Bb%d" % i, [128, 16, 128], BF16) for i in range(2)]; bBb = [Buf(), Buf()]
                Bp = [sb("Bp%d" % i, [128, 16, 128], BF16) for i in range(2)]; bBp = [Buf(), Buf()]
                NU = 3
                u4 = [sb("u4_%d" % i, [128, 4, 1024], BF16) for i in range(NU)]; bu4 = [Buf() for _ in range(NU)]; gu4 = [P.grp("u4") for _ in range(NU)]
                v4 = [sb("v4_%d" % i, [128, 4, 1024], BF16) for i in range(NU)]; bv4 = [Buf() for _ in range(NU)]; gv4 = [P.grp("v4") for _ in range(NU)]
                NG = 4
                gl = [sb("gl%d" % i, [128, TT], F32) for i in range(NG)]; bgl = [Buf() for _ in range(NG)]
                Wt = [sb("Wt%d" % i, [128, TT], BF16) for i in range(NG)]; bWt = [Buf() for _ in range(NG)]
                io3 = io128[:].unsqueeze(1).to_broadcast([128, 16, 128])
                ucnt = 0
                kcnt = 0
                gcnt = 0
                for tile in range(S // TT):
                    t0 = tile * TT
                    s = tile % 2
                    def load_tile(tl_):
                        s_ = tl_ % 2
                        tt0 = tl_ * TT
                        P.dma("sp", ghn2[s_], hn[s_][:], hnT_s[:, :, tt0:tt0 + TT].rearrange("c p t -> p c t"), writes=[bhn2[s_]])
                        P.dma("sp", grt2[s_], rt[s_][:], rT_s[:, :, tt0:tt0 + TT].rearrange("i p t -> p i t"), writes=[brt2[s_]])
                        P.dma("sp", ghh[s_], hh[s_][:], y[tt0:tt0 + TT, :].rearrange("(b p) f -> p b f", p=128), writes=[bhh[s_]])
                    if tile == 0:
                        load_tile(0)
                    for sub in range(TT // 16):
                        c0 = sub * 16
                        a = sub % 2
                        tt("dve", Ab[a][:], io3, rt[s][:, 0, c0:c0 + 16].unsqueeze(2).to_broadcast([128, 16, 128]), ALU.is_equal,
                           [bio, brt2[s]], [bAb[a]])
                        tt("dve", Bb[a][:], io3, rt[s][:, 1, c0:c0 + 16].unsqueeze(2).to_broadcast([128, 16, 128]), ALU.is_equal,
                           [bio, brt2[s]], [bBb[a]])
                        tt("pool", Bp[a][:], Bb[a][:], rt[s][:, 2, c0:c0 + 16].unsqueeze(2).to_broadcast([128, 16, 128]), ALU.mult,
                           [bBb[a], brt2[s]], [bBp[a]])
                        for c4 in range(4):
                            bank = 6 + (gcnt % 2)
                            gcnt += 1
                            for c in range(4):
                                cc = c4 * 4 + c
                                mm(PS[bank][:, :].rearrange("p (i c) -> p c i", c=4)[:, c, :], Bp[a][:, cc, :], Ab[a][:, cc, :], True, True,
                                   [bBp[a], bAb[a]], [PSB[bank]])
                            tk = c0 + c4 * 4
                            act(Gt[:, :, tk:tk + 4], PS[bank][:, :].rearrange("p (i c) -> p i c", c=4), AF.Copy,
                                [PSB[bank]], [bGt])
                    def vside(j, k, us, jj):
                        for b in range(2):
                            for hf in range(2):
                                mm(PS[b * 2 + hf][:, :], Wt[k][:, b * 128:(b + 1) * 128], v4[us][:, jj, hf * 512:(hf + 1) * 512],
                                   j == 0, j == 127, [bWt[k], bv4[us]], [PSB[b * 2 + hf]])
                    pend = None
                    for jg in range(32):
                        us = ucnt % NU
                        ucnt += 1
                        P.dma("sp", gu4[us], u4[us][:], uT_s[jg * 4:(jg + 1) * 4].rearrange("j p f -> p j f"), writes=[bu4[us]])
                        P.dma("sp", gv4[us], v4[us][:], v_s[jg * 512:(jg + 1) * 512, :].rearrange("(j n) f -> n j f", n=128), writes=[bv4[us]])
                        if jg == 12 and tile + 1 < S // TT:
                            load_tile(tile + 1)
                        for jj in range(4):
                            j = jg * 4 + jj
                            abank = 4 + (j % 4)
                            k = kcnt % NG
                            kcnt += 1
                            for c in range(8):
                                mm(PS[abank][:, 0:TT], u4[us][:, jj, c * 128:(c + 1) * 128], hn[s][:, c, :], c == 0, c == 7,
                                   [bu4[us], bhn2[s]], [PSB[abank]])
                            act(gl[k][:], PS[abank][:, 0:TT], GELU, [PSB[abank]], [bgl[k]])
                            tt("dve", Wt[k][:], gl[k][:], Gt[:, j, :], ALU.mult, [bgl[k], bGt], [bWt[k]])
                            if pend is not None:
                                vside(*pend)
                            pend = (j, k, us, jj)
                    vside(*pend)
                    pend = None
                    for b in range(2):
                        for hf in range(2):
                            tt("dve", hh[s][:, b, hf * 512:(hf + 1) * 512], PS[b * 2 + hf][:, :], hh[s][:, b, hf * 512:(hf + 1) * 512], ALU.add,
                               [PSB[b * 2 + hf], bhh[s]], [bhh[s]])
                    P.dma("pool", gho[s], y[t0:t0 + TT, :].rearrange("(b p) f -> p b f", p=128), hh[s][:], reads=[bhh[s]])
                P.barrier()

        P.emit()
    return nc


def prep_shared(norm1_g, w_in, q_norm_g, k_norm_g, w_pool, pool_scale, w_out, norm2_g, w_query, sub_keys, expert_u, expert_v):
    f = np.float32
    w_in0 = np.asarray(w_in[0], f)
    w_in_r = np.ascontiguousarray(w_in0.reshape(8, 128, 60, 128).transpose(2, 1, 0, 3))
    w_out_r = np.ascontiguousarray(np.asarray(w_out[0], f).reshape(8, 128, 1024).transpose(1, 0, 2))
    w_q_r = np.ascontiguousarray(np.asarray(w_query[0], f).reshape(8, 128, 2048).transpose(1, 0, 2))
    w_pool_r = np.ascontiguousarray(np.asarray(w_pool[0], f).transpose(1, 0, 2))
    skT = np.ascontiguousarray(np.asarray(sub_keys[0], f).reshape(16, 128, 128).transpose(2, 0, 1))
    uT_r = np.ascontiguousarray(np.asarray(expert_u[0], f).reshape(128, 128, 8, 128).transpose(0, 3, 2, 1))
    v_in = np.ascontiguousarray(np.asarray(expert_v[0], f))
    vecs = np.zeros((128, 32), f)
    vecs[:, 0:8] = np.asarray(norm1_g[0], f).reshape(8, 128).T
    vecs[:, 8:16] = np.asarray(norm2_g[0], f).reshape(8, 128).T
    vecs[:, 16:24] = np.asarray(pool_scale[0], f).reshape(8, 128).T
    vecs[:, 24] = np.tile(np.asarray(q_norm_g[0], f), 2)
    vecs[:, 25] = np.tile(np.asarray(k_norm_g[0], f), 2)
    return dict(w_in_r=w_in_r, w_out_r=w_out_r, w_q_r=w_q_r, w_pool_r=w_pool_r, skT=skT, uT_r=uT_r, v_in=v_in, vecs=vecs)


_NC_CACHE = {}


def kernel(x, norm1_g, w_in, q_norm_g, k_norm_g, w_pool, pool_scale, w_out, norm2_g, w_query, sub_keys, expert_u, expert_v):
    x = np.asarray(x, np.float32)
    B, S, D = x.shape
    shared = prep_shared(norm1_g, w_in, q_norm_g, k_norm_g, w_pool, pool_scale, w_out, norm2_g, w_query, sub_keys, expert_u, expert_v)
    if S not in _NC_CACHE:
        _NC_CACHE[S] = build(S)
    nc = _NC_CACHE[S]
    in_maps = []
    for b in range(B):
        m = dict(shared)
        m["x"] = np.ascontiguousarray(x[b])
        in_maps.append(m)
    res = run_bass_kernel_spmd(nc, in_maps, core_ids=list(range(B)))
    return np.stack([np.asarray(r["y"], np.float32) for r in res.results], axis=0)
```

```python
import numpy as np
from contextlib import ExitStack
import concourse.bass as bass
import concourse.mybir as mybir
from concourse.bass_utils import run_bass_kernel_spmd

F32 = mybir.dt.float32
BF16 = mybir.dt.bfloat16
U32 = mybir.dt.uint32
I32 = mybir.dt.int32
AF = mybir.ActivationFunctionType
ALU = mybir.AluOpType
AX = mybir.AxisListType

ENGS = ("pe", "dve", "act", "pool", "sp")
EPOCH = 20000


class Buf:
    __slots__ = ("w", "r", "name")

    def __init__(self, name=""):
        self.w = {}
        self.r = {}
        self.name = name


class Grp:
    __slots__ = ("key", "cnt")

    def __init__(self, key):
        self.key = key
        self.cnt = 0


class Prog:
    def __init__(self, nc, stack):
        self.nc = nc
        self.stack = stack
        self.semh = []
        self.ops = {e: [] for e in ENGS}
        self.cnt = {e: 0 for e in ENGS}
        self.key = {}
        self.ownkeys = {e: set() for e in ENGS}
        self.waited = {e: {} for e in ENGS}
        for e in ENGS:
            self._new_epoch(e)
        self.nops = 0
        self.latest = {}

    def barrier(self):
        for e in ENGS:
            waits = []
            for k, v in self.latest.items():
                if k in self.ownkeys[e]:
                    continue
                if self.waited[e].get(k, 0) >= v:
                    continue
                self.waited[e][k] = v
                waits.append((k, v))
            self.ops[e].append((waits, None, None))

    def _new_sem(self, name):
        h = self.stack.enter_context(self.nc.semaphore(name))
        self.semh.append(h)
        return len(self.semh) - 1

    def _new_epoch(self, e):
        k = self._new_sem("s_%s_%d" % (e, len(self.ownkeys[e])))
        self.key[e] = k
        self.ownkeys[e].add(k)
        self.cnt[e] = 0

    def grp(self, name="g"):
        return Grp(self._new_sem("d_%s_%d" % (name, len(self.semh))))

    def sb(self, name, shape, dt):
        return self.stack.enter_context(self.nc.sbuf_tensor(name, shape, dt))

    def ps(self, name, shape, dt):
        return self.stack.enter_context(self.nc.psum_tensor(name, shape, dt))

    def _deps(self, eng, reads, writes):
        deps = {}
        for b in reads:
            for k, v in b.w.items():
                if deps.get(k, 0) < v:
                    deps[k] = v
        for b in writes:
            for k, v in b.w.items():
                if deps.get(k, 0) < v:
                    deps[k] = v
            for k, v in b.r.items():
                if deps.get(k, 0) < v:
                    deps[k] = v
        waits = []
        wd = self.waited[eng]
        for k, v in deps.items():
            if eng == "pe" and k in self.ownkeys[eng]:
                continue
            if wd.get(k, 0) >= v:
                continue
            wd[k] = v
            waits.append((k, v))
        return waits

    def _mark(self, tok, reads, writes):
        k, v = tok
        self.latest[k] = v
        for b in reads:
            b.r[k] = v
        for b in writes:
            b.w = {k: v}
            b.r = {}

    def op(self, eng, fn, reads=(), writes=()):
        waits = self._deps(eng, reads, writes)
        if self.cnt[eng] >= EPOCH:
            self._new_epoch(eng)
        self.cnt[eng] += 1
        tok = (self.key[eng], self.cnt[eng])
        self.ops[eng].append((waits, fn, (tok[0], 1)))
        self._mark(tok, reads, writes)
        self.nops += 1
        return tok

    def dma(self, eng, grp, out, in_, reads=(), writes=(), **kw):
        waits = self._deps(eng, reads, writes)
        grp.cnt += 16
        tok = (grp.key, grp.cnt)
        self.ops[eng].append((waits, lambda e: e.dma_start(out=out, in_=in_, **kw), (tok[0], 16)))
        self._mark(tok, reads, writes)
        self.nops += 1
        return tok

    def wait_all(self, eng, bufs):
        waits = self._deps(eng, bufs, bufs)
        self.ops[eng].append((waits, None, None))

    def emit(self):
        nc = self.nc
        semh = self.semh
        ops = self.ops

        def replay(name, e):
            for waits, fn, inc in ops[name]:
                for k, v in waits:
                    e.wait_ge(semh[k], v)
                if fn is not None:
                    ins = fn(e)
                    ins.then_inc(semh[inc[0]], inc[1])

        with nc.Block() as block:
            @block.tensor
            def _(e):
                replay("pe", e)

            @block.vector
            def _(e):
                replay("dve", e)

            @block.scalar
            def _(e):
                replay("act", e)

            @block.gpsimd
            def _(e):
                replay("pool", e)

            @block.sync
            def _(e):
                replay("sp", e)


def _mk(P):
    def mm(out, lhsT, rhs, start, stop, reads, writes):
        return P.op("pe", lambda e: e.matmul(out, lhsT=lhsT, rhs=rhs, start=start, stop=stop), reads, writes)
    def tr(out, in_, ident, reads, writes):
        return P.op("pe", lambda e: e.transpose(out=out, in_=in_, identity=ident), reads, writes)
    def act(out, in_, func, reads, writes, **kw):
        return P.op("act", lambda e: e.activation(out=out, in_=in_, func=func, **kw), reads, writes)
    def tt(eng, out, in0, in1, op, reads, writes):
        return P.op(eng, lambda e: e.tensor_tensor(out=out, in0=in0, in1=in1, op=op), reads, writes)
    def ts(eng, out, in0, s1, s2, op0, op1, reads, writes):
        if s2 is None:
            return P.op(eng, lambda e: e.tensor_scalar(out=out, in0=in0, scalar1=s1, scalar2=None, op0=op0), reads, writes)
        return P.op(eng, lambda e: e.tensor_scalar(out=out, in0=in0, scalar1=s1, scalar2=s2, op0=op0, op1=op1), reads, writes)
    def stt(eng, out, in0, scalar, in1, op0, op1, reads, writes):
        return P.op(eng, lambda e: e.scalar_tensor_tensor(out=out, in0=in0, scalar=scalar, in1=in1, op0=op0, op1=op1), reads, writes)
    def cp(eng, out, in_, reads, writes):
        if eng == "act":
            return P.op(eng, lambda e: e.copy(out=out, in_=in_), reads, writes)
        return P.op(eng, lambda e: e.tensor_copy(out=out, in_=in_), reads, writes)
    def ms(eng, ap, val, writes):
        return P.op(eng, lambda e: e.memset(ap, val), (), writes)
    def red(eng, out, in_, op, reads, writes):
        return P.op(eng, lambda e: e.tensor_reduce(out=out, in_=in_, axis=AX.X, op=op), reads, writes)
    P.mm, P.tr, P.act, P.tt, P.ts, P.stt, P.cp, P.ms, P.red = mm, tr, act, tt, ts, stt, cp, ms, red
    return P

import math

EPS = 1e-6
PATS = ((128, 1), (512, 4), (2048, 16))


def alibi_slopes(n):
    def geometric(k):
        start = 2.0 ** (-8.0 / k)
        return [start ** (i + 1) for i in range(k)]
    p = 2 ** int(math.floor(math.log2(n)))
    s = geometric(p) + geometric(2 * p)[0::2][: n - p]
    return np.sort(np.array(s, dtype=np.float32))[::-1].copy()


def build(S=8192, debug=False, phases="ABCD", gelu_func=None):
    nc = bass.Bass("TRN2", target_bir_lowering=False)
    GELU = gelu_func or AF.Gelu_apprx_tanh
    HALF = min(S, 4096)
    NH = S // HALF
    TPH = HALF // 512
    NSB = S // 2048
    skind = "ExternalOutput" if debug else "Internal"

    def din(name, shape, dt=F32):
        return nc.dram_tensor(name, shape, dt, kind="ExternalInput").ap()

    def dscr(name, shape, dt):
        return nc.dram_tensor(name, shape, dt, kind=skind).ap()

    x = din("x", [S, 1024])
    w_in_r = din("w_in_r", [60, 128, 8, 128])
    w_out_r = din("w_out_r", [128, 8, 1024])
    w_q_r = din("w_q_r", [128, 8, 2048])
    w_pool_r = din("w_pool_r", [128, 4, 256])
    skT_in = din("skT", [128, 16, 128])
    uT_r = din("uT_r", [128, 128, 8, 128])
    v_in = din("v_in", [16384, 1024])
    vecs = din("vecs", [128, 32])
    y = nc.dram_tensor("y", [S, 1024], F32, kind="ExternalOutput").ap()

    qT = dscr("qT", [24, 128, S], BF16)
    kT = [dscr("kT%d" % p, [8, 128, S], BF16) for p in range(3)]
    g1T = dscr("g1T", [8, 128, S], BF16)
    pgT = dscr("pgT", [8, 128, S], BF16)
    vaug = dscr("vaug", [S, 2048], BF16)
    mT = dscr("mT", [8, 128, S], BF16)
    hnT_s = dscr("hnT_s", [8, 128, S], BF16)
    rT_s = dscr("rT_s", [3, 128, S], F32)
    uT_s = dscr("uT_s", [128, 128, 1024], BF16)
    v_s = dscr("v_s", [16384, 1024], BF16)

    slopes = alibi_slopes(48).reshape(3, 16)

    with ExitStack() as st:
        P = _mk(Prog(nc, st))
        mm, tr, act, tt, ts, stt, cp, ms, red = P.mm, P.tr, P.act, P.tt, P.ts, P.stt, P.cp, P.ms, P.red

        vec = P.sb("vec", [128, 32], F32); bvec = Buf()
        identf = P.sb("identf", [128, 128], F32); bidf = Buf()
        identb = P.sb("identb", [128, 128], BF16); bidb = Buf()
        bd = P.sb("bd", [128, 128], BF16); bbd = Buf()
        io128 = P.sb("io128", [128, 128], F32); bio = Buf()
        gq = P.sb("gq", [128, 2], F32); bgq = Buf()
        epsc = P.sb("epsc", [128, 1], F32); beps = Buf()
        P.op("pool", lambda e: e.memset(epsc[:], EPS), (), [beps])
        gc = P.grp("c")
        P.dma("sp", gc, vec[:], vecs, writes=[bvec])
        ms("pool", identf[:], 0.0, [bidf])
        P.op("pool", lambda e: e.affine_select(out=identf[:], in_=identf[:], pattern=[[-1, 128]], compare_op=ALU.not_equal,
                                               fill=1.0, base=0, channel_multiplier=1), [bidf], [bidf])
        cp("pool", identb[:], identf[:], [bidf], [bidb])
        ms("pool", bd[:], 0.0, [bbd])
        ms("pool", bd[0:64, 0:64], 1.0 / 64, [bbd])
        ms("pool", bd[64:128, 64:128], 1.0 / 64, [bbd])
        P.op("pool", lambda e: e.iota(io128[:], pattern=[[1, 128]], base=0, channel_multiplier=0,
                                      allow_small_or_imprecise_dtypes=True), (), [bio])
        ts("dve", gq[:, 0:1], vec[:, 24:25], 0.125, None, ALU.mult, None, [bvec], [bgq])
        cp("dve", gq[:, 1:2], vec[:, 25:26], [bvec, bgq], [bgq])
        g1col = vec[:, 0:8]; g2col = vec[:, 8:16]; pscol = vec[:, 16:24]

        PS = [P.ps("ps%d" % i, [128, 512], F32) for i in range(8)]
        PSB = [Buf("ps%d" % i) for i in range(8)]

        def norm_transpose(src_ap, bsrc, ssq, bssq, junk, bjunk, rstd, brstd, xs, bxs, ptr_bank, gcol, dst_ap, bdst):
            ms("pool", ssq, 0.0, [bssq])
            act(junk, src_ap, AF.Square, [bsrc, bssq], [bjunk, bssq], accum_out=ssq)
            act(rstd, ssq, AF.Sqrt, [bssq, beps], [brstd], scale=1.0 / 1024, bias=epsc[:])
            P.op("dve", lambda e: e.reciprocal(out=rstd, in_=rstd), [brstd], [brstd])
            act(xs, src_ap, AF.Copy, [bsrc, brstd], [bxs], scale=rstd)
            pt = PS[ptr_bank].bitcast(BF16)
            for c in range(8):
                tr(pt[:, c * 128:(c + 1) * 128], xs[:, c * 128:(c + 1) * 128], identb[:], [bxs, bidb], [PSB[ptr_bank]])
            tt("dve", dst_ap, pt[:, 0:1024].rearrange("p (c t) -> p c t", c=8),
               gcol.unsqueeze(2).to_broadcast([128, 8, 128]), ALU.mult, [PSB[ptr_bank], bvec], [bdst])

        if "A" in phases:
            with ExitStack() as sa:
                def sb(name, shape, dt):
                    return sa.enter_context(nc.sbuf_tensor("a_" + name, shape, dt))
                xnT = sb("xnT", [128, 8, HALF], BF16); bxn = Buf()
                xts = [sb("xt%d" % i, [128, 1024], F32) for i in range(2)]; bxt = [Buf(), Buf()]; gxt = [P.grp("x"), P.grp("x")]
                junk = sb("junk", [128, 1024], F32); bjunk = Buf()
                ssq = sb("ssq", [128, 1], F32); bssq = Buf()
                rstd1 = sb("rstd1", [128, 1], F32); brstd1 = Buf()
                xs = sb("xs", [128, 1024], BF16); bxs = Buf()
                wst = [sb("wst%d" % i, [128, 8, 128], F32) for i in range(2)]; bwst = [Buf(), Buf()]; gwst = [P.grp("w"), P.grp("w")]
                wbf = [sb("wbf%d" % i, [128, 8, 128], BF16) for i in range(2)]; bwbf = [Buf(), Buf()]
                wv = sb("wv", [128, 8, 1024], BF16); bwv = Buf()
                wpst = sb("wpst", [128, 4, 256], F32); bwpst = Buf()
                wpb = sb("wpb", [128, 4, 256], BF16); bwpb = Buf()
                NST = 4
                stg = [sb("stg%d" % i, [128, 2048], BF16) for i in range(NST)]; bstg = [Buf() for _ in range(NST)]
                gstg = [P.grp("st") for _ in range(NST)]
                kst = [sb("kst%d" % i, [128, 2048], BF16) for i in range(2)]; bkst = [Buf(), Buf()]; gkst = [P.grp("k"), P.grp("k")]
                sq = [sb("sq%d" % i, [128, 512], BF16) for i in range(2)]; bsq = [Buf(), Buf()]
                rs = [sb("rs%d" % i, [128, 512], F32) for i in range(2)]; brs = [Buf(), Buf()]
                abuf = sb("abuf", [128, 528], F32); bab = Buf()
                sA = sb("sA", [128, 528], F32); bsA = Buf()
                sB = sb("sB", [128, 528], F32); bsB = Buf()
                dT = sb("dT", [128, HALF], BF16); bdT = Buf()
                ahalo = sb("ahalo", [128, 4, 16], F32); bah = Buf()
                rc = sb("rc", [128, 4, 16], F32); brc = Buf()
                tmp16 = sb("tmp16", [128, 16], F32); btmp16 = Buf()
                g0s = [sb("g0s%d" % i, [128, 512], BF16) for i in range(2)]; bg0s = [Buf(), Buf()]
                vst = [sb("vst%d" % i, [128, 2048], BF16) for i in range(2)]; bvst = [Buf(), Buf()]; gvst = [P.grp("v"), P.grp("v")]
                cst = [sb("cst%d" % i, [128, 2048], F32) for i in range(2)]; bcst = [Buf(), Buf()]; gcst = [P.grp("cs"), P.grp("cs")]
                cbf = [sb("cbf%d" % i, [128, 2048], BF16) for i in range(2)]; bcbf = [Buf(), Buf()]; gcbf = [P.grp("cb"), P.grp("cb")]

                gA = P.grp("A")
                P.dma("sp", gA, wpst[:], w_pool_r, writes=[bwpst])
                cp("pool", wpb[:], wpst[:], [bwpst], [bwpb])
                ms("pool", ahalo[:], 0.0, [bah])
                for g in range(4):
                    w = PATS and (2, 4, 8, 16)[g]
                    ts("pool", rc[:, g, :], io128[:, 0:16], 1.0, float(w), ALU.add, ALU.min, [bio], [brc])
                P.op("dve", lambda e: e.reciprocal(out=rc[:], in_=rc[:]), [brc], [brc])
                for i in range(2):
                    ms("pool", vst[i][:], 1.0, [bvst[i]])

                p0_items = []
                if "D" in phases:
                    for it in range(64):
                        p0_items.append(("u", it))
                        p0_items.append(("v", it))
                p0_state = {"i": 0}

                def p0_step():
                    i = p0_state["i"]
                    if i >= len(p0_items):
                        return
                    p0_state["i"] = i + 1
                    kind, it = p0_items[i]
                    s = i % 2
                    if kind == "u":
                        src = uT_r[it * 2:(it + 1) * 2].rearrange("j p c n -> p j (c n)")
                        dst = uT_s[it * 2:(it + 1) * 2].rearrange("j p f -> p j f")
                    else:
                        src = v_in[it * 256:(it + 1) * 256, :].rearrange("(j n) f -> n j f", n=128)
                        dst = v_s[it * 256:(it + 1) * 256, :].rearrange("(j n) f -> n j f", n=128)
                    P.dma("sp", gcst[s], cst[s][:].rearrange("p (j f) -> p j f", j=2), src, writes=[bcst[s]])
                    cp("pool" if (i % 4) < 2 else "act", cbf[s][:], cst[s][:], [bcst[s]], [bcbf[s]])
                    P.dma("pool", gcbf[s], dst, cbf[s][:].rearrange("p (j f) -> p j f", j=2), reads=[bcbf[s]])

                mmcnt = [0]
                stgcnt = [0]
                pendA = [None]
                for half in range(NH):
                    h0 = half * HALF
                    for b in range(HALF // 128):
                        s = b % 2
                        t0 = h0 + b * 128
                        P.dma("sp", gxt[s], xts[s][:], x[t0:t0 + 128, :], writes=[bxt[s]])
                        norm_transpose(xts[s][:], bxt[s], ssq[:], bssq, junk[:], bjunk, rstd1[:], brstd1, xs[:], bxs, 7, g1col,
                                       xnT[:, :, b * 128:(b + 1) * 128], bxn)
                    for k in range(8):
                        s = k % 2
                        P.dma("sp", gwst[s], wst[s][:], w_in_r[36 + k], writes=[bwst[s]])
                        cp("pool", wv[:, :, k * 128:(k + 1) * 128], wst[s][:], [bwst[s]], [bwv])
                    for b in range(HALF // 128):
                        s = b % 2
                        t0 = h0 + b * 128
                        vv = vst[s][:].rearrange("p (q j e) -> p q j e", q=8, j=2)
                        for hf in range(2):
                            bank = 5 + hf
                            for c in range(8):
                                mm(PS[bank][:, :], xnT[:, c, b * 128:(b + 1) * 128], wv[:, c, hf * 512:(hf + 1) * 512],
                                   c == 0, c == 7, [bxn, bwv], [PSB[bank]])
                            pv = PS[bank][:, :].rearrange("p (q j e) -> p q j e", q=4, j=2)
                            act(vv[:, hf * 4:(hf + 1) * 4, 0, 0:64], pv[:, :, 0, :], AF.Copy, [PSB[bank]], [bvst[s]])
                            act(vv[:, hf * 4:(hf + 1) * 4, 1, 64:128], pv[:, :, 1, :], AF.Copy, [PSB[bank]], [bvst[s]])
                        P.dma("pool", gvst[s], vaug[t0:t0 + 128, :], vst[s][:], reads=[bvst[s]])

                    order = []
                    for g in range(4):
                        order.append(("a", g, g))
                        for oc in range(2):
                            order.append(("y", 44 + g * 2 + oc, g * 2 + oc))
                    for qc in range(24):
                        order.append(("q", 4 + qc, qc))
                    for kc in range(8):
                        order.append(("k", 28 + kc, kc))
                    for c in range(8):
                        order.append(("g1", 52 + c, c))
                    def load_w(oi_):
                        ws_ = oi_ % 2
                        P.dma("sp", gwst[ws_], wst[ws_][:], w_in_r[order[oi_][1]], writes=[bwst[ws_]])
                        cp("pool", wbf[ws_][:], wst[ws_][:], [bwst[ws_]], [bwbf[ws_]])
                    load_w(0)
                    for oi, (typ, fc, idx) in enumerate(order):
                        ws = oi % 2
                        if oi + 1 < len(order):
                            load_w(oi + 1)
                        p0_step()
                        for tti in range(TPH):
                            bank = (0, 1, 4, 5, 6)[mmcnt[0] % 5]
                            mmcnt[0] += 1
                            tsl = slice(tti * 512, (tti + 1) * 512)
                            for c in range(8):
                                mm(PS[bank][:, :], wbf[ws][:, c, :], xnT[:, c, tsl], c == 0, c == 7, [bwbf[ws], bxn], [PSB[bank]])
                            def post_tile(typ=typ, idx=idx, tti=tti, bank=bank, half=half, h0=h0, tsl=tsl):
                                pp = PS[bank][:, :]
                                tl = tti % 4
                                sbi = (h0 // 2048) + tti // 4
                                if typ == "a":
                                    g = idx
                                    wsz = (2, 4, 8, 16)[g]
                                    if tti == 0:
                                        cp("pool", abuf[:, 0:16], ahalo[:, g, :], [bah], [bab])
                                    act(abuf[:, 16:528], pp, AF.Copy, [PSB[bank]], [bab])
                                    cur, bcur = abuf, bab
                                    srcs = [(sA, bsA), (sB, bsB)]
                                    off = 0
                                    for stp in range(g + 1):
                                        sh = 1 << stp
                                        off += sh
                                        dst, bdst = srcs[stp % 2]
                                        tt("pool", dst[:, off:528], cur[:, off:528], cur[:, off - sh:528 - sh], ALU.add, [bcur], [bdst])
                                        cur, bcur = dst, bdst
                                    stt("dve", dT[:, tsl], cur[:, 16:528], 1.0 / wsz, abuf[:, 16:528], ALU.mult, ALU.subtract,
                                        [bcur, bab], [bdT])
                                    if half == 0 and tti == 0:
                                        tt("pool", tmp16[:], cur[:, 16:32], rc[:, g, :], ALU.mult, [bcur, brc], [btmp16])
                                        tt("pool", dT[:, 0:16], tmp16[:], abuf[:, 16:32], ALU.subtract, [btmp16, bab], [bdT])
                                    if tti == TPH - 1:
                                        cp("pool", ahalo[:, g, :], abuf[:, 512:528], [bab], [bah])
                                    else:
                                        cp("pool", abuf[:, 0:16], abuf[:, 512:528], [bab], [bab])
                                elif typ == "y":
                                    yc = idx
                                    g, oc = yc // 2, yc % 2
                                    gs = tti % 2
                                    act(g0s[gs][:], pp, AF.Sigmoid, [PSB[bank]], [bg0s[gs]])
                                    ybank = 2 + (tti % 2)
                                    mm(PS[ybank][:, :], wpb[:, g, oc * 128:(oc + 1) * 128], dT[:, tsl], True, True, [bwpb, bdT], [PSB[ybank]])
                                    si = stgcnt[0] % NST
                                    stt("dve", stg[si][:, tl * 512:(tl + 1) * 512], PS[ybank][:, :], pscol[:, yc:yc + 1], g0s[gs][:],
                                        ALU.mult, ALU.mult, [PSB[ybank], bg0s[gs], bvec], [bstg[si]])
                                    if tl == 3:
                                        P.dma("pool", gstg[si], pgT[yc, :, sbi * 2048:(sbi + 1) * 2048], stg[si][:], reads=[bstg[si]])
                                        stgcnt[0] += 1
                                elif typ in ("q", "k"):
                                    ss_ = tti % 2
                                    act(sq[ss_][:], pp, AF.Square, [PSB[bank]], [bsq[ss_]])
                                    sbank = 2 + (tti % 2)
                                    mm(PS[sbank][:, :], bd[:], sq[ss_][:], True, True, [bbd, bsq[ss_]], [PSB[sbank]])
                                    act(rs[ss_][:], PS[sbank][:, :], AF.Sqrt, [PSB[sbank], beps], [brs[ss_]], bias=epsc[:])
                                    P.op("dve", (lambda t: (lambda e: e.reciprocal(out=t, in_=t)))(rs[ss_][:]), [brs[ss_]], [brs[ss_]])
                                    gcol = gq[:, 0:1] if typ == "q" else gq[:, 1:2]
                                    if typ == "q":
                                        p = idx // 8
                                        d = PATS[p][1]
                                        si = stgcnt[0] % NST
                                        if d == 1:
                                            o_ap = stg[si][:, tl * 512:(tl + 1) * 512]
                                            i0_ap, i1_ap = pp, rs[ss_][:]
                                        elif d == 4:
                                            o_ap = stg[si][:, tl * 512:(tl + 1) * 512].rearrange("p (r i) -> p i r", r=4)
                                            i0_ap = pp.rearrange("p (i r) -> p i r", r=4)
                                            i1_ap = rs[ss_][:].rearrange("p (i r) -> p i r", r=4)
                                        else:
                                            o_ap = stg[si][:, :].rearrange("p (r i) -> p i r", r=16)[:, tl * 32:(tl + 1) * 32, :]
                                            i0_ap = pp.rearrange("p (i r) -> p i r", r=16)
                                            i1_ap = rs[ss_][:].rearrange("p (i r) -> p i r", r=16)
                                        stt("dve", o_ap, i0_ap, gcol, i1_ap, ALU.mult, ALU.mult, [PSB[bank], brs[ss_], bgq], [bstg[si]])
                                        if tl == 3:
                                            P.dma("pool", gstg[si], qT[idx, :, sbi * 2048:(sbi + 1) * 2048], stg[si][:], reads=[bstg[si]])
                                            stgcnt[0] += 1
                                    else:
                                        si = stgcnt[0] % NST
                                        nat = stg[si][:, tl * 512:(tl + 1) * 512]
                                        stt("dve", nat, pp, gcol, rs[ss_][:], ALU.mult, ALU.mult, [PSB[bank], brs[ss_], bgq], [bstg[si]])
                                        cp("pool", kst[0][:, tl * 512:(tl + 1) * 512].rearrange("p (r i) -> p i r", r=4),
                                           nat.rearrange("p (i r) -> p i r", r=4), [bstg[si]], [bkst[0]])
                                        cp("pool", kst[1][:, :].rearrange("p (r i) -> p i r", r=16)[:, tl * 32:(tl + 1) * 32, :],
                                           nat.rearrange("p (i r) -> p i r", r=16), [bstg[si]], [bkst[1]])
                                        if tl == 3:
                                            ssl = slice(sbi * 2048, (sbi + 1) * 2048)
                                            P.dma("pool", gstg[si], kT[0][idx, :, ssl], stg[si][:], reads=[bstg[si]])
                                            P.dma("pool", gkst[0], kT[1][idx, :, ssl], kst[0][:], reads=[bkst[0]])
                                            P.dma("pool", gkst[1], kT[2][idx, :, ssl], kst[1][:], reads=[bkst[1]])
                                            stgcnt[0] += 1
                                else:
                                    si = stgcnt[0] % NST
                                    act(stg[si][:, tl * 512:(tl + 1) * 512], pp, AF.Sigmoid, [PSB[bank]], [bstg[si]])
                                    if tl == 3:
                                        P.dma("pool", gstg[si], g1T[idx, :, sbi * 2048:(sbi + 1) * 2048], stg[si][:], reads=[bstg[si]])
                                        stgcnt[0] += 1
                            if pendA[0] is not None:
                                pendA[0]()
                            pendA[0] = post_tile
                    if pendA[0] is not None:
                        pendA[0]()
                        pendA[0] = None
                while p0_state["i"] < len(p0_items):
                    p0_step()
                P.barrier()

        if "B" in phases:
            with ExitStack() as sa:
                def sb(name, shape, dt):
                    return sa.enter_context(nc.sbuf_tensor("b_" + name, shape, dt))
                M = sb("M", [128, 48, 256], BF16); bM = Buf()
                tq = sb("tq", [128, 128], F32); btq = Buf()
                tA = sb("tA", [128, 128], F32); btA = Buf()
                tB = sb("tB", [128, 128], F32); btB = Buf()
                tC = sb("tC", [128, 128], F32); btC = Buf()
                P.op("pool", lambda e: e.iota(tq[:], pattern=[[1, 128]], base=0, channel_multiplier=-1,
                                              allow_small_or_imprecise_dtypes=True), (), [btq])
                for p in range(3):
                    d = PATS[p][1]
                    for h in range(16):
                        c = float(slopes[p, h]) * d
                        ph = p * 16 + h
                        ts("pool", tA[:], tq[:], 0.0, None, ALU.max, None, [btq], [btA])
                        act(tB[:], tA[:], AF.Exp, [btA], [btB], scale=-c)
                        P.op("pool", lambda e: e.affine_select(out=tC[:], in_=tB[:], pattern=[[1, 128]], compare_op=ALU.is_ge,
                                                               fill=0.0, base=0, channel_multiplier=-1), [btB], [btC])
                        cp("dve", M[:, ph, 128:256], tC[:], [btC], [bM])
                        ts("pool", tA[:], tq[:], 0.0, 128.0, ALU.min, ALU.add, [btq], [btA])
                        act(tB[:], tA[:], AF.Exp, [btA], [btB], scale=-c)
                        P.op("pool", lambda e: e.affine_select(out=tC[:], in_=tB[:], pattern=[[-1, 128]], compare_op=ALU.is_ge,
                                                               fill=0.0, base=0, channel_multiplier=1), [btB], [btC])
                        cp("dve", M[:, ph, 0:128], tC[:], [btC], [bM])
                q3 = sb("q3", [128, 3, 2048], BF16); bq3 = Buf(); gq3 = P.grp("q3")
                k3 = [sb("k3_%d" % i, [128, 3, 2048], BF16) for i in range(2)]; bk3 = [Buf(), Buf()]; gk3 = [P.grp("k3"), P.grp("k3")]
                va = [sb("va_%d" % i, [128, 3, 16, 256], BF16) for i in range(2)]; bva = [Buf(), Buf()]; gva = [P.grp("va"), P.grp("va")]
                g1c = sb("g1c", [128, 2048], BF16); bg1c = Buf(); gg1 = P.grp("g1c")
                pgc = sb("pgc", [128, 2048], BF16); bpgc = Buf(); gpg = P.grp("pgc")
                Oacc = [sb("Oacc%d" % i, [128, 2048], F32) for i in range(2)]; bOa = [Buf(), Buf()]
                att = sb("att", [128, 2048], F32); batt = Buf()
                rd = sb("rd", [128, 2048], F32); brd = Buf()
                mc = sb("mc", [128, 2048], BF16); bmc = Buf(); gmc = P.grp("mc")
                NE = 3
                E = [sb("E%d" % i, [128, 512], F32) for i in range(NE)]; bE = [Buf() for _ in range(NE)]
                PT = [sb("PT%d" % i, [128, 512], BF16) for i in range(NE)]; bPT = [Buf() for _ in range(NE)]
                ecnt = [0]
                ocnt = [0]

                def vrecip_b(out, in_, reads, writes):
                    return P.op("dve", lambda e: e.reciprocal(out=out, in_=in_), reads, writes)
                for hp in range(8):
                    for sbi in range(NSB):
                        cur = sbi % 2
                        prv = 1 - cur
                        ssl = slice(sbi * 2048, (sbi + 1) * 2048)
                        for p in range(3):
                            P.dma("sp", gq3, q3[:, p, :], qT[p * 8 + hp, :, ssl], writes=[bq3])
                            P.dma("sp", gk3[cur], k3[cur][:, p, :], kT[p][hp, :, ssl], writes=[bk3[cur]])
                            d = PATS[p][1]
                            nb = 16 // d
                            if d == 1:
                                src = vaug[sbi * 2048:(sbi + 1) * 2048, hp * 256:(hp + 1) * 256].rearrange("(n i) f -> i n f", i=128)
                                P.dma("sp", gva[cur], va[cur][:, p, :, :], src, writes=[bva[cur]])
                            else:
                                for n in range(nb):
                                    r0 = sbi * 2048 + n * 128 * d
                                    src = vaug[r0:r0 + 128 * d, hp * 256:(hp + 1) * 256].rearrange("(i r) f -> i r f", r=d)
                                    P.dma("sp", gva[cur], va[cur][:, p, n * d:(n + 1) * d, :], src, writes=[bva[cur]])
                        P.dma("sp", gg1, g1c[:], g1T[hp, :, ssl], writes=[bg1c])
                        P.dma("sp", gpg, pgc[:], pgT[hp, :, ssl], writes=[bpgc])
                        def make_item(hh, p, grp4, cp2, obank, sbank, ei, cur, prv, sbi, hp):
                            h = hp * 2 + hh
                            rows = slice(hh * 64, hh * 64 + 64)
                            d = PATS[p][1]
                            ph = p * 16 + h
                            pS = PS[sbank][:, :].rearrange("p (c j q) -> p c j q", c=2, j=2)
                            Ev = E[ei][:].rearrange("p (c j q) -> p c j q", c=2, j=2)
                            PTv = PT[ei][:].rearrange("p (c j q) -> p c j q", c=2, j=2)
                            infos = []
                            for j in range(2):
                                cl = grp4 * 4 + cp2 * 2 + j
                                if cl >= d:
                                    infos.append((cl, k3[cur][rows, p, (cl - d) * 128:(cl - d + 1) * 128],
                                                  va[cur][:, p, cl - d, hh * 128:(hh + 1) * 128], bk3[cur], bva[cur]))
                                elif sbi > 0:
                                    infos.append((cl, k3[prv][rows, p, (cl + 16 - d) * 128:(cl + 17 - d) * 128],
                                                  va[prv][:, p, cl + 16 - d, hh * 128:(hh + 1) * 128], bk3[prv], bva[prv]))
                                else:
                                    infos.append((cl, None, None, None, None))

                            def qk():
                                for j in range(2):
                                    cl, kp_ap, vp_ap, bkp, bvp = infos[j]
                                    qap = q3[rows, p, cl * 128:(cl + 1) * 128]
                                    kc_ap = k3[cur][rows, p, cl * 128:(cl + 1) * 128]
                                    mm(pS[:, j, 1, :], kc_ap, qap, True, True, [bk3[cur], bq3], [PSB[sbank]])
                                    if kp_ap is not None:
                                        mm(pS[:, j, 0, :], kp_ap, qap, True, True, [bkp, bq3], [PSB[sbank]])

                            def post():
                                allprev = all(i[1] is not None for i in infos)
                                if allprev:
                                    act(E[ei][:], PS[sbank][:, :], AF.Exp, [PSB[sbank]], [bE[ei]])
                                    tt("dve", PTv, Ev, M[:, ph, :].rearrange("p (j q) -> p j q", j=2).unsqueeze(1).to_broadcast([128, 2, 2, 128]),
                                       ALU.mult, [bE[ei], bM], [bPT[ei]])
                                else:
                                    act(Ev[:, :, 1, :], pS[:, :, 1, :], AF.Exp, [PSB[sbank]], [bE[ei]])
                                    tt("dve", PTv[:, :, 1, :], Ev[:, :, 1, :], M[:, ph, 128:256].unsqueeze(1).to_broadcast([128, 2, 128]),
                                       ALU.mult, [bE[ei], bM], [bPT[ei]])
                                    for j in range(2):
                                        if infos[j][1] is not None:
                                            act(Ev[:, j, 0, :], pS[:, j, 0, :], AF.Exp, [PSB[sbank]], [bE[ei]])
                                            tt("dve", PTv[:, j, 0, :], Ev[:, j, 0, :], M[:, ph, 0:128], ALU.mult, [bE[ei], bM], [bPT[ei]])

                            def pv():
                                pO = PS[obank][:, :].rearrange("p (c q) -> p c q", c=4)
                                for j in range(2):
                                    cl, kp_ap, vp_ap, bkp, bvp = infos[j]
                                    oj = cp2 * 2 + j
                                    vc_ap = va[cur][:, p, cl, hh * 128:(hh + 1) * 128]
                                    if vp_ap is not None:
                                        mm(pO[:, oj, :], vp_ap, PTv[:, j, 0, :], True, False, [bvp, bPT[ei]], [PSB[obank]])
                                        mm(pO[:, oj, :], vc_ap, PTv[:, j, 1, :], False, True, [bva[cur], bPT[ei]], [PSB[obank]])
                                    else:
                                        mm(pO[:, oj, :], vc_ap, PTv[:, j, 1, :], True, True, [bva[cur], bPT[ei]], [PSB[obank]])
                                if cp2 == 1:
                                    if d == 1:
                                        oap = Oacc[hh][:, grp4 * 512:(grp4 + 1) * 512].rearrange("p (c q) -> p c q", c=4)
                                    elif d == 4:
                                        oap = Oacc[hh][:, grp4 * 512:(grp4 + 1) * 512].rearrange("p (i r) -> p r i", r=4)
                                    else:
                                        oap = Oacc[hh][:, :].rearrange("p (i r) -> p r i", r=16)[:, grp4 * 4:(grp4 + 1) * 4, :]
                                    if p == 0:
                                        cp("dve", oap, pO, [PSB[obank]], [bOa[hh]])
                                    else:
                                        tt("dve", oap, oap, pO, ALU.add, [PSB[obank], bOa[hh]], [bOa[hh]])
                                if p == 2 and grp4 == 3 and cp2 == 1:
                                    if hh == 0:
                                        num, den = Oacc[0][0:64, :], Oacc[0][64:128, :]
                                    else:
                                        num, den = Oacc[1][64:128, :], Oacc[1][0:64, :]
                                    vrecip_b(rd[rows, :], den, [bOa[hh]], [brd])
                                    tt("dve", att[rows, :], num, rd[rows, :], ALU.mult, [bOa[hh], brd], [batt])
                                    tt("pool", att[rows, :], att[rows, :], g1c[rows, :], ALU.mult, [batt, bg1c], [batt])
                                    tt("pool", mc[rows, :], att[rows, :], pgc[rows, :], ALU.add, [batt, bpgc], [bmc])
                            return qk, post, pv

                        items = []
                        for hh in range(2):
                            for p in range(3):
                                for grp4 in range(4):
                                    obank = 3 + (ocnt[0] % 2)
                                    ocnt[0] += 1
                                    for cp2 in range(2):
                                        sbank = ecnt[0] % 3
                                        ei = ecnt[0] % NE
                                        ecnt[0] += 1
                                        items.append(make_item(hh, p, grp4, cp2, obank, sbank, ei, cur, prv, sbi, hp))
                        for ii, (qk, post, pv) in enumerate(items):
                            qk()
                            post()
                            if ii > 0:
                                items[ii - 1][2]()
                        items[-1][2]()
                        P.dma("pool", gmc, mT[hp, :, ssl], mc[:], reads=[bmc])
                P.barrier()

        def vmax(out, in_, reads, writes):
            return P.op("dve", lambda e: e.max(out=out, in_=in_), reads, writes)

        def vmaxidx(out, in_max, in_values, reads, writes):
            return P.op("dve", lambda e: e.max_index(out=out, in_max=in_max, in_values=in_values), reads, writes)

        def vmr(out, rep, vals, reads, writes):
            return P.op("dve", lambda e: e.match_replace(out=out, in_to_replace=rep, in_values=vals, imm_value=-1e30), reads, writes)

        def vrecip(out, in_, reads, writes):
            return P.op("dve", lambda e: e.reciprocal(out=out, in_=in_), reads, writes)

        if "C" in phases:
            with ExitStack() as sa:
                def sb(name, shape, dt):
                    return sa.enter_context(nc.sbuf_tensor("c_" + name, shape, dt))
                woutb = sb("woutb", [128, 8, 1024], BF16); bwo = Buf()
                wqb = sb("wqb", [128, 8, 2048], BF16); bwq = Buf()
                skb = sb("skb", [128, 16, 128], BF16); bsk = Buf()
                wl = [sb("wl%d" % i, [128, 2048], F32) for i in range(2)]; bwl = [Buf(), Buf()]; gwl = [P.grp("wl"), P.grp("wl")]
                pieces = []
                for i in range(4):
                    pieces.append((w_out_r[:, 2 * i:2 * i + 2, :].rearrange("p c f -> p (c f)"),
                                   woutb[:, 2 * i:2 * i + 2, :].rearrange("p c f -> p (c f)"), bwo))
                for i in range(8):
                    pieces.append((w_q_r[:, i, :], wqb[:, i, :], bwq))
                pieces.append((skT_in.rearrange("p h n -> p (h n)"), skb[:].rearrange("p h n -> p (h n)"), bsk))
                for i, (src, dst, bd_) in enumerate(pieces):
                    s = i % 2
                    P.dma("sp", gwl[s], wl[s][:], src, writes=[bwl[s]])
                    cp("pool" if i % 2 else "act", dst, wl[s][:], [bwl[s]], [bd_])
                mt = [sb("mt%d" % i, [128, 8, 512], BF16) for i in range(2)]; bmt = [Buf(), Buf()]; gmt = [P.grp("mt"), P.grp("mt")]
                xt2 = [sb("xt2_%d" % i, [128, 1024], F32) for i in range(2)]; bxt2 = [Buf(), Buf()]; gxt2 = [P.grp("x2"), P.grp("x2")]
                ht = [sb("ht%d" % i, [128, 1024], F32) for i in range(2)]; bht = [Buf(), Buf()]; ght = [P.grp("ht"), P.grp("ht")]
                junk = sb("junk", [128, 1024], F32); bjunk = Buf()
                ssq = sb("ssq", [128, 1], F32); bssq = Buf()
                rstd1 = sb("rstd1", [128, 1], F32); brstd1 = Buf()
                xs = sb("xs", [128, 1024], BF16); bxs = Buf()
                hnst = [sb("hnst%d" % i, [128, 8, 512], BF16) for i in range(2)]; bhn = [Buf(), Buf()]; ghn = [P.grp("hn"), P.grp("hn")]
                rtst = [sb("rtst%d" % i, [128, 3, 512], F32) for i in range(2)]; brt = [Buf(), Buf()]; grt = [P.grp("rt"), P.grp("rt")]
                qpT = sb("qpT", [128, 16, 128], BF16); bqp = Buf()
                scs = [sb("sc%d" % i, [128, 16, 128], F32) for i in range(2)]; bscs = [Buf(), Buf()]
                sc2 = sb("sc2", [128, 16, 128], F32); bsc2 = Buf()
                ts1 = sb("ts1", [128, 16, 16], F32); bts1 = Buf()
                ti1 = sb("ti1", [128, 16, 16], U32); bti1 = Buf()
                tif = sb("tif", [128, 16, 16], F32); btif = Buf()
                cand = sb("cand", [128, 8, 256], F32); bcand = Buf()
                cand2 = sb("cand2", [128, 8, 256], F32); bcand2 = Buf()
                bs = sb("bs", [128, 8, 16], F32); bbs = Buf()
                bj = sb("bj", [128, 8, 16], U32); bbj = Buf()
                bjf = sb("bjf", [128, 8, 16], F32); bbjf = Buf()
                ba = sb("ba", [128, 8, 16], F32); bba = Buf()
                bb_ = sb("bb", [128, 8, 16], F32); bbb = Buf()
                oh = sb("oh", [128, 8, 16, 16], F32); boh = Buf()
                prod = sb("prod", [128, 8, 16, 16], F32); bprod = Buf()
                ri = sb("ri", [128, 3, 128], F32); bri = Buf()
                ex = sb("ex", [128, 8, 16], F32); bex = Buf()
                sm = sb("sm", [128, 8], F32); bsm = Buf()
                io16b = io128[:, 0:16].unsqueeze(1).unsqueeze(1).to_broadcast([128, 8, 16, 16])
                thr = sb("thr", [128, 16], F32); bthr = Buf()
                P.op("pool", lambda e: e.iota(thr[:], pattern=[[16, 16]], base=16, channel_multiplier=0,
                                              allow_small_or_imprecise_dtypes=True), (), [bthr])
                thrb = thr[:].unsqueeze(1).unsqueeze(1).to_broadcast([128, 8, 16, 16])
                def c_front(grp_i, b):
                    sc = scs[(grp_i * 4 + b) % 2]; bsc = bscs[(grp_i * 4 + b) % 2]
                    gs = grp_i % 2
                    tg0 = grp_i * 512
                    if b == 0:
                        P.dma("sp", gmt[gs], mt[gs][:], mT[:, :, tg0:tg0 + 512].rearrange("c p t -> p c t"), writes=[bmt[gs]])
                    blk = grp_i * 4 + b
                    s = blk % 2
                    t0 = tg0 + b * 128
                    bsl = slice(b * 128, (b + 1) * 128)
                    P.dma("sp", gxt2[s], xt2[s][:], x[t0:t0 + 128, :], writes=[bxt2[s]])
                    for hf in range(2):
                        for c in range(8):
                            mm(PS[hf][:, :], mt[gs][:, c, bsl], woutb[:, c, hf * 512:(hf + 1) * 512], c == 0, c == 7,
                               [bmt[gs], bwo], [PSB[hf]])
                    for hf in range(2):
                        tt("dve", ht[s][:, hf * 512:(hf + 1) * 512], PS[hf][:, :], xt2[s][:, hf * 512:(hf + 1) * 512], ALU.add,
                           [PSB[hf], bxt2[s]], [bht[s]])
                    P.dma("pool", ght[s], y[t0:t0 + 128, :], ht[s][:], reads=[bht[s]])
                    norm_transpose(ht[s][:], bht[s], ssq[:], bssq, junk[:], bjunk, rstd1[:], brstd1, xs[:], bxs, 7, g2col,
                                   hnst[gs][:, :, bsl], bhn[gs])
                    for j in range(16):
                        bank = 2 + j // 4
                        for c in range(8):
                            mm(PS[bank][:, (j % 4) * 128:(j % 4 + 1) * 128], wqb[:, c, j * 128:(j + 1) * 128], hnst[gs][:, c, bsl],
                               c == 0, c == 7, [bwq, bhn[gs]], [PSB[bank]])
                    for g4 in range(4):
                        act(qpT[:, g4 * 4:(g4 + 1) * 4, :].rearrange("p j t -> p (j t)"), PS[2 + g4][:, :], AF.Copy, [PSB[2 + g4]], [bqp])
                    for j in range(16):
                        bank = 2 + j // 4
                        mm(PS[bank][:, (j % 4) * 128:(j % 4 + 1) * 128], qpT[:, j, :], skb[:, j, :], True, True, [bqp, bsk], [PSB[bank]])
                    for g4 in range(4):
                        act(sc[:, g4 * 4:(g4 + 1) * 4, :].rearrange("p j t -> p (j t)"), PS[2 + g4][:, :], AF.Copy, [PSB[2 + g4]], [bsc])

                def c_back(grp_i, b):
                    sc = scs[(grp_i * 4 + b) % 2]; bsc = bscs[(grp_i * 4 + b) % 2]
                    gs = grp_i % 2
                    tg0 = grp_i * 512
                    blk = grp_i * 4 + b
                    bsl = slice(b * 128, (b + 1) * 128)
                    for j in range(16):
                        vmax(ts1[:, j, 0:8], sc[:, j, :], [bsc], [bts1])
                        vmaxidx(ti1[:, j, 0:8], ts1[:, j, 0:8], sc[:, j, :], [bsc, bts1], [bti1])
                        vmr(sc2[:, j, :], ts1[:, j, 0:8], sc[:, j, :], [bsc, bts1], [bsc2])
                        vmax(ts1[:, j, 8:16], sc2[:, j, :], [bsc2], [bts1])
                        vmaxidx(ti1[:, j, 8:16], ts1[:, j, 8:16], sc2[:, j, :], [bsc2, bts1], [bti1])
                    cp("dve", tif[:], ti1[:], [bti1], [btif])
                    ts1v = ts1[:].rearrange("p (h t) k -> p h t k", t=2)
                    tifv = tif[:].rearrange("p (h t) k -> p h t k", t=2)
                    tt("dve", cand[:].rearrange("p h (a b) -> p h a b", a=16),
                       ts1v[:, :, 0, :].unsqueeze(3).to_broadcast([128, 8, 16, 16]),
                       ts1v[:, :, 1, :].unsqueeze(2).to_broadcast([128, 8, 16, 16]), ALU.add, [bts1], [bcand])
                    for h in range(8):
                        vmax(bs[:, h, 0:8], cand[:, h, :], [bcand], [bbs])
                        vmaxidx(bj[:, h, 0:8], bs[:, h, 0:8], cand[:, h, :], [bcand, bbs], [bbj])
                        vmr(cand2[:, h, :], bs[:, h, 0:8], cand[:, h, :], [bcand, bbs], [bcand2])
                        vmax(bs[:, h, 8:16], cand2[:, h, :], [bcand2], [bbs])
                        vmaxidx(bj[:, h, 8:16], bs[:, h, 8:16], cand2[:, h, :], [bcand2, bbs], [bbj])
                    cp("dve", bjf[:], bj[:], [bbj], [bbjf])
                    tt("dve", oh[:], bjf[:].unsqueeze(3).to_broadcast([128, 8, 16, 16]), thrb, ALU.is_ge, [bbjf, bthr], [boh])
                    red("dve", ba[:], oh[:], ALU.add, [boh], [bba])
                    stt("dve", bb_[:], ba[:], -16.0, bjf[:], ALU.mult, ALU.add, [bba, bbjf], [bbb])
                    riv = ri[:].rearrange("p i (h k) -> p i h k", h=8)
                    for half_i, (src_t, bsrc_t) in enumerate(((ba, bba), (bb_, bbb))):
                        tt("dve", oh[:], src_t[:].unsqueeze(3).to_broadcast([128, 8, 16, 16]), io16b, ALU.is_equal, [bsrc_t, bio], [boh])
                        tt("dve", prod[:], oh[:], tifv[:, :, half_i, :].unsqueeze(2).to_broadcast([128, 8, 16, 16]), ALU.mult,
                           [boh, btif], [bprod])
                        red("dve", riv[:, half_i, :, :], prod[:], ALU.add, [bprod], [bri])
                    tt("dve", ex[:], bs[:], bs[:, :, 0:1].to_broadcast([128, 8, 16]), ALU.subtract, [bbs], [bex])
                    act(ex[:], ex[:], AF.Exp, [bex], [bex])
                    red("dve", sm[:], ex[:], ALU.add, [bex], [bsm])
                    vrecip(sm[:], sm[:], [bsm], [bsm])
                    tt("dve", riv[:, 2, :, :], ex[:], sm[:].unsqueeze(2).to_broadcast([128, 8, 16]), ALU.mult, [bex, bsm], [bri])
                    for i in range(3):
                        tr(PS[6][:, i * 128:(i + 1) * 128], ri[:, i, :], identf[:], [bri, bidf], [PSB[6]])
                    act(rtst[gs][:, :, bsl], PS[6][:, 0:384].rearrange("p (i t) -> p i t", i=3), AF.Copy, [PSB[6]], [brt[gs]])
                    if b == 3:
                        P.dma("pool", ghn[gs], hnT_s[:, :, tg0:tg0 + 512].rearrange("c p t -> p c t"), hnst[gs][:], reads=[bhn[gs]])
                        P.dma("pool", grt[gs], rT_s[:, :, tg0:tg0 + 512].rearrange("i p t -> p i t"), rtst[gs][:], reads=[brt[gs]])

                blocks = [(g_, b_) for g_ in range(S // 512) for b_ in range(4)]
                c_front(*blocks[0])
                for bi_ in range(len(blocks)):
                    if bi_ + 1 < len(blocks):
                        c_front(*blocks[bi_ + 1])
                    c_back(*blocks[bi_])
                P.barrier()

        if "D" in phases:
            with ExitStack() as sa:
                def sb(name, shape, dt):
                    return sa.enter_context(nc.sbuf_tensor("d_" + name, shape, dt))
                TT = 256
                Gt = sb("Gt", [128, 128, TT], BF16); bGt = Buf()
                hn = [sb("hn%d" % i, [128, 8, TT], BF16) for i in range(2)]; bhn2 = [Buf(), Buf()]; ghn2 = [P.grp("hn2"), P.grp("hn2")]
                rt = [sb("rt%d" % i, [128, 3, TT], F32) for i in range(2)]; brt2 = [Buf(), Buf()]; grt2 = [P.grp("rt2"), P.grp("rt2")]
                hh = [sb("hh%d" % i, [128, 2, 1024], F32) for i in range(2)]; bhh = [Buf(), Buf()]; ghh = [P.grp("hh"), P.grp("hh")]
                gho = [P.grp("ho"), P.grp("ho")]
                Ab = [sb("Ab%d" % i, [128, 16, 128], BF16) for i in range(2)]; bAb = [Buf(), Buf()]
                Bb = [sb("Bb%d" % i, [128, 16, 128], BF16) for i in range(2)]; bBb = [Buf(), Buf()]
                Bp = [sb("Bp%d" % i, [128, 16, 128], BF16) for i in range(2)]; bBp = [Buf(), Buf()]
                NU = 3
                u4 = [sb("u4_%d" % i, [128, 4, 1024], BF16) for i in range(NU)]; bu4 = [Buf() for _ in range(NU)]; gu4 = [P.grp("u4") for _ in range(NU)]
                v4 = [sb("v4_%d" % i, [128, 4, 1024], BF16) for i in range(NU)]; bv4 = [Buf() for _ in range(NU)]; gv4 = [P.grp("v4") for _ in range(NU)]
                NG = 3
                gl = [sb("gl%d" % i, [128, TT], F32) for i in range(NG)]; bgl = [Buf() for _ in range(NG)]
                Wt = [sb("Wt%d" % i, [128, TT], BF16) for i in range(NG)]; bWt = [Buf() for _ in range(NG)]
                io3 = io128[:].unsqueeze(1).to_broadcast([128, 16, 128])
                ucnt = 0
                kcnt = 0
                gcnt = 0
                for tile in range(S // TT):
                    t0 = tile * TT
                    s = tile % 2
                    P.dma("sp", ghn2[s], hn[s][:], hnT_s[:, :, t0:t0 + TT].rearrange("c p t -> p c t"), writes=[bhn2[s]])
                    P.dma("sp", grt2[s], rt[s][:], rT_s[:, :, t0:t0 + TT].rearrange("i p t -> p i t"), writes=[brt2[s]])
                    P.dma("sp", ghh[s], hh[s][:], y[t0:t0 + TT, :].rearrange("(b p) f -> p b f", p=128), writes=[bhh[s]])
                    for sub in range(TT // 16):
                        c0 = sub * 16
                        a = sub % 2
                        tt("dve", Ab[a][:], io3, rt[s][:, 0, c0:c0 + 16].unsqueeze(2).to_broadcast([128, 16, 128]), ALU.is_equal,
                           [bio, brt2[s]], [bAb[a]])
                        tt("dve", Bb[a][:], io3, rt[s][:, 1, c0:c0 + 16].unsqueeze(2).to_broadcast([128, 16, 128]), ALU.is_equal,
                           [bio, brt2[s]], [bBb[a]])
                        tt("pool", Bp[a][:], Bb[a][:], rt[s][:, 2, c0:c0 + 16].unsqueeze(2).to_broadcast([128, 16, 128]), ALU.mult,
                           [bBb[a], brt2[s]], [bBp[a]])
                        for c4 in range(4):
                            bank = 6 + (gcnt % 2)
                            gcnt += 1
                            for c in range(4):
                                cc = c4 * 4 + c
                                mm(PS[bank][:, :].rearrange("p (i c) -> p c i", c=4)[:, c, :], Bp[a][:, cc, :], Ab[a][:, cc, :], True, True,
                                   [bBp[a], bAb[a]], [PSB[bank]])
                            tk = c0 + c4 * 4
                            act(Gt[:, :, tk:tk + 4], PS[bank][:, :].rearrange("p (i c) -> p i c", c=4), AF.Copy,
                                [PSB[bank]], [bGt])
                    def vside(j, k, us, jj):
                        for b in range(2):
                            for hf in range(2):
                                mm(PS[b * 2 + hf][:, :], Wt[k][:, b * 128:(b + 1) * 128], v4[us][:, jj, hf * 512:(hf + 1) * 512],
                                   j == 0, j == 127, [bWt[k], bv4[us]], [PSB[b * 2 + hf]])
                    pend = None
                    for jg in range(32):
                        us = ucnt % NU
                        ucnt += 1
                        P.dma("sp", gu4[us], u4[us][:], uT_s[jg * 4:(jg + 1) * 4].rearrange("j p f -> p j f"), writes=[bu4[us]])
                        P.dma("sp", gv4[us], v4[us][:], v_s[jg * 512:(jg + 1) * 512, :].rearrange("(j n) f -> n j f", n=128), writes=[bv4[us]])
                        for jj in range(4):
                            j = jg * 4 + jj
                            abank = 4 + (j % 2)
                            k = kcnt % NG
                            kcnt += 1
                            for c in range(8):
                                mm(PS[abank][:, 0:TT], u4[us][:, jj, c * 128:(c + 1) * 128], hn[s][:, c, :], c == 0, c == 7,
                                   [bu4[us], bhn2[s]], [PSB[abank]])
                            act(gl[k][:], PS[abank][:, 0:TT], GELU, [PSB[abank]], [bgl[k]])
                            tt("dve", Wt[k][:], gl[k][:], Gt[:, j, :], ALU.mult, [bgl[k], bGt], [bWt[k]])
                            if pend is not None:
                                vside(*pend)
                            pend = (j, k, us, jj)
                    vside(*pend)
                    pend = None
                    for b in range(2):
                        for hf in range(2):
                            tt("dve", hh[s][:, b, hf * 512:(hf + 1) * 512], PS[b * 2 + hf][:, :], hh[s][:, b, hf * 512:(hf + 1) * 512], ALU.add,
                               [PSB[b * 2 + hf], bhh[s]], [bhh[s]])
                    P.dma("pool", gho[s], y[t0:t0 + TT, :].rearrange("(b p) f -> p b f", p=128), hh[s][:], reads=[bhh[s]])
                P.barrier()

        P.emit()
    return nc


def prep_shared(norm1_g, w_in, q_norm_g, k_norm_g, w_pool, pool_scale, w_out, norm2_g, w_query, sub_keys, expert_u, expert_v):
    f = np.float32
    w_in0 = np.asarray(w_in[0], f)
    w_in_r = np.ascontiguousarray(w_in0.reshape(8, 128, 60, 128).transpose(2, 1, 0, 3))
    w_out_r = np.ascontiguousarray(np.asarray(w_out[0], f).reshape(8, 128, 1024).transpose(1, 0, 2))
    w_q_r = np.ascontiguousarray(np.asarray(w_query[0], f).reshape(8, 128, 2048).transpose(1, 0, 2))
    w_pool_r = np.ascontiguousarray(np.asarray(w_pool[0], f).transpose(1, 0, 2))
    skT = np.ascontiguousarray(np.asarray(sub_keys[0], f).reshape(16, 128, 128).transpose(2, 0, 1))
    uT_r = np.ascontiguousarray(np.asarray(expert_u[0], f).reshape(128, 128, 8, 128).transpose(0, 3, 2, 1))
    v_in = np.ascontiguousarray(np.asarray(expert_v[0], f))
    vecs = np.zeros((128, 32), f)
    vecs[:, 0:8] = np.asarray(norm1_g[0], f).reshape(8, 128).T
    vecs[:, 8:16] = np.asarray(norm2_g[0], f).reshape(8, 128).T
    vecs[:, 16:24] = np.asarray(pool_scale[0], f).reshape(8, 128).T
    vecs[:, 24] = np.tile(np.asarray(q_norm_g[0], f), 2)
    vecs[:, 25] = np.tile(np.asarray(k_norm_g[0], f), 2)
    return dict(w_in_r=w_in_r, w_out_r=w_out_r, w_q_r=w_q_r, w_pool_r=w_pool_r, skT=skT, uT_r=uT_r, v_in=v_in, vecs=vecs)


_NC_CACHE = {}


def kernel(x, norm1_g, w_in, q_norm_g, k_norm_g, w_pool, pool_scale, w_out, norm2_g, w_query, sub_keys, expert_u, expert_v):
    x = np.asarray(x, np.float32)
    B, S, D = x.shape
    shared = prep_shared(norm1_g, w_in, q_norm_g, k_norm_g, w_pool, pool_scale, w_out, norm2_g, w_query, sub_keys, expert_u, expert_v)
    if S not in _NC_CACHE:
        _NC_CACHE[S] = build(S)
    nc = _NC_CACHE[S]
    in_maps = []
    for b in range(B):
        m = dict(shared)
        m["x"] = np.ascontiguousarray(x[b])
        in_maps.append(m)
    res = run_bass_kernel_spmd(nc, in_maps, core_ids=list(range(B)))
    return np.stack([np.asarray(r["y"], np.float32) for r in res.results], axis=0)
```
